# Optimizing a Trainium2 kernel written in Bass

```python
import math
import jax, jax.numpy as jnp
from jax import lax
import numpy as np

D_MODEL = 1024
BATCH = 8
SEQ = 4096
DEPTH = 2

N_A = max(DEPTH // 2, 1)
N_B = DEPTH - N_A
N_DENSE = (DEPTH + 1) // 2
N_MOE = DEPTH // 2

POOL_WINDOWS = (2, 4, 8, 16)
N_POOL_GROUPS = len(POOL_WINDOWS)
POOL_GROUP_DIM = D_MODEL // N_POOL_GROUPS
POOL_MAX = max(POOL_WINDOWS)

HEAD_DIM = 128
N_HEADS = D_MODEL // HEAD_DIM
BLOCK = 256
TOP_BLOCKS = 3
Q_CHUNK = 16
ROT_DIM = HEAD_DIM // 4
ROPE_THETA = 500000.0

D_FF = 2816
N_EXPERTS = 8
TOP_K = 2
D_FF_EXPERT = 3584

EPS = 1e-6

kernel_name = "yoco_pool_moba_moe_hybrid"


def rms_norm(x, g):
    xf = x.astype(jnp.float32)
    var = jnp.mean(xf * xf, axis=-1, keepdims=True)
    return (xf * lax.rsqrt(var + EPS) * g.astype(jnp.float32)).astype(x.dtype)


def rope_partial(x, pos):
    half = ROT_DIM // 2
    inv = ROPE_THETA ** (-jnp.arange(half, dtype=jnp.float32) * 2.0 / ROT_DIM)
    ang = pos.astype(jnp.float32)[:, None] * inv[None, :]
    cos, sin = jnp.cos(ang), jnp.sin(ang)
    xf = x.astype(jnp.float32)
    x1 = xf[..., :half]
    x2 = xf[..., half:ROT_DIM]
    out = jnp.concatenate([x1 * cos - x2 * sin, x2 * cos + x1 * sin, xf[..., ROT_DIM:]], axis=-1)
    return out.astype(x.dtype)


def pool_mixer(x, w_pool, scale):
    B, S, D = x.shape
    xf = x.astype(jnp.float32)
    csp = jnp.pad(jnp.cumsum(xf, axis=1), ((0, 0), (POOL_MAX, 0), (0, 0)))
    t = jnp.arange(S)
    groups = []
    for g, w in enumerate(POOL_WINDOWS):
        sl = slice(g * POOL_GROUP_DIM, (g + 1) * POOL_GROUP_DIM)
        win_sum = csp[:, POOL_MAX:POOL_MAX + S, sl] - csp[:, POOL_MAX - w:POOL_MAX - w + S, sl]
        count = jnp.minimum(t + 1, w).astype(jnp.float32)[None, :, None]
        groups.append(win_sum / count - xf[:, :, sl])
    pooled = jnp.stack(groups, axis=2)
    y = jnp.einsum('bsgc,gcd->bsgd', pooled, w_pool.astype(jnp.float32))
    y = y.reshape(B, S, D) * scale.astype(jnp.float32)
    return y.astype(x.dtype)


def swiglu(x, wg, wu, wd):
    return (jax.nn.silu(x @ wg) * (x @ wu)) @ wd


def moe_swiglu(x, w_router, wg, wu, wd):
    B, S, D = x.shape
    t = x.reshape(B * S, D)
    logits = (t @ w_router).astype(jnp.float32)
    top_v, top_i = lax.top_k(logits, TOP_K)
    top_w = jax.nn.softmax(top_v, axis=-1)
    gates = jnp.sum(jax.nn.one_hot(top_i, N_EXPERTS, dtype=jnp.float32) * top_w[..., None], axis=1)
    y = jnp.zeros((B * S, D), jnp.float32)
    for e in range(N_EXPERTS):
        ye = swiglu(t, wg[e], wu[e], wd[e]).astype(jnp.float32)
        y = y + gates[:, e:e + 1] * ye
    return y.astype(x.dtype).reshape(B, S, D)


def shared_kv(h, kv_norm, wk, wv, pos):
    B, S, D = h.shape
    hn = rms_norm(h, kv_norm)
    k = (hn @ wk).reshape(B, S, N_HEADS, HEAD_DIM).transpose(0, 2, 1, 3)
    v = (hn @ wv).reshape(B, S, N_HEADS, HEAD_DIM).transpose(0, 2, 1, 3)
    k = rope_partial(k, pos)
    n_blocks = -(-S // BLOCK)
    pad = n_blocks * BLOCK - S
    k = jnp.pad(k, ((0, 0), (0, 0), (0, pad), (0, 0)))
    v = jnp.pad(v, ((0, 0), (0, 0), (0, pad), (0, 0)))
    kb = k.reshape(B, N_HEADS, n_blocks, BLOCK, HEAD_DIM)
    vb = v.reshape(B, N_HEADS, n_blocks, BLOCK, HEAD_DIM)
    k_mean = jnp.mean(kb.astype(jnp.float32), axis=3)
    return kb, vb, k_mean


def moba_attention(q, kb, vb, k_mean):
    B, H, S, hd = q.shape
    n_blocks = kb.shape[2]
    ks = min(TOP_BLOCKS, n_blocks)
    scale = hd ** -0.5
    q_blk = jnp.arange(S) // BLOCK
    gate = jnp.einsum('bhsd,bhnd->bhsn', q.astype(jnp.float32), k_mean)
    fully_past = jnp.arange(n_blocks)[None, :] < q_blk[:, None]
    gate = jnp.where(fully_past[None, None], gate, -jnp.inf)
    _, sel = lax.top_k(gate, ks)
    slot_valid = jnp.arange(ks)[None, :] < q_blk[:, None]
    bi = jnp.arange(B)[:, None, None, None]
    hi = jnp.arange(H)[None, :, None, None]

    def chunk(c):
        start = c * Q_CHUNK
        qc = lax.dynamic_slice_in_dim(q, start, Q_CHUNK, axis=2)
        selc = lax.dynamic_slice_in_dim(sel, start, Q_CHUNK, axis=2)
        validc = lax.dynamic_slice_in_dim(slot_valid, start, Q_CHUNK, axis=0)
        kg = kb[bi, hi, selc]
        vg = vb[bi, hi, selc]
        s_sel = jnp.einsum('bhqd,bhqnkd->bhqnk', qc, kg).astype(jnp.float32) * scale
        s_sel = jnp.where(validc[None, None, :, :, None], s_sel, -jnp.inf)
        ob = start // BLOCK
        ko = lax.dynamic_index_in_dim(kb, ob, axis=2, keepdims=False)
        vo = lax.dynamic_index_in_dim(vb, ob, axis=2, keepdims=False)
        s_own = jnp.einsum('bhqd,bhkd->bhqk', qc, ko).astype(jnp.float32) * scale
        kpos = ob * BLOCK + jnp.arange(BLOCK)
        qpos = start + jnp.arange(Q_CHUNK)
        s_own = jnp.where((kpos[None, :] <= qpos[:, None])[None, None], s_own, -jnp.inf)
        logits = jnp.concatenate([s_sel.reshape(B, H, Q_CHUNK, ks * BLOCK), s_own], axis=-1)
        p = jax.nn.softmax(logits, axis=-1).astype(vb.dtype)
        p_sel = p[..., :ks * BLOCK].reshape(B, H, Q_CHUNK, ks, BLOCK)
        p_own = p[..., ks * BLOCK:]
        return (jnp.einsum('bhqnk,bhqnkd->bhqd', p_sel, vg)
                + jnp.einsum('bhqk,bhkd->bhqd', p_own, vo))

    outs = lax.map(chunk, jnp.arange(S // Q_CHUNK))
    return outs.transpose(1, 2, 0, 3, 4).reshape(B, H, S, hd)


def setup_inputs(seed: int = 0) -> dict:
    key = jax.random.key(seed)
    ks = jax.random.split(key, 20)
    D = D_MODEL
    f32 = jnp.float32
    nrm = lambda k, shape, fan: jax.random.normal(k, shape, f32) * (fan ** -0.5)
    return {
        "x": jax.random.normal(ks[0], (BATCH, SEQ, D), f32),
        "attn_norm": 1.0 + 0.02 * jax.random.normal(ks[1], (DEPTH, D), f32),
        "ffn_norm": 1.0 + 0.02 * jax.random.normal(ks[2], (DEPTH, D), f32),
        "pool_w": nrm(ks[3], (N_A, N_POOL_GROUPS, POOL_GROUP_DIM, POOL_GROUP_DIM), POOL_GROUP_DIM),
        "pool_scale": 1.0 + 0.02 * jax.random.normal(ks[4], (N_A, D), f32),
        "wq": nrm(ks[5], (N_B, D, D), D),
        "wo": nrm(ks[6], (N_B, D, D), D),
        "kv_norm": 1.0 + 0.02 * jax.random.normal(ks[7], (D,), f32),
        "wk": nrm(ks[8], (D, D), D),
        "wv": nrm(ks[9], (D, D), D),
        "ffn_w_gate": nrm(ks[10], (N_DENSE, D, D_FF), D),
        "ffn_w_up": nrm(ks[11], (N_DENSE, D, D_FF), D),
        "ffn_w_down": nrm(ks[12], (N_DENSE, D_FF, D), D_FF),
        "router_w": nrm(ks[13], (N_MOE, D, N_EXPERTS), D),
        "moe_w_gate": nrm(ks[14], (N_MOE, N_EXPERTS, D, D_FF_EXPERT), D),
        "moe_w_up": nrm(ks[15], (N_MOE, N_EXPERTS, D, D_FF_EXPERT), D),
        "moe_w_down": nrm(ks[16], (N_MOE, N_EXPERTS, D_FF_EXPERT, D), D_FF_EXPERT),
        "final_norm": 1.0 + 0.02 * jax.random.normal(ks[17], (D,), f32),
    }


def reference(x, attn_norm, ffn_norm, pool_w, pool_scale, wq, wo, kv_norm, wk, wv,
              ffn_w_gate, ffn_w_up, ffn_w_down, router_w, moe_w_gate, moe_w_up, moe_w_down,
              final_norm):
    B, S, D = x.shape
    pos = jnp.arange(S)
    h = x
    kb = vb = k_mean = None
    for l in range(DEPTH):
        hn = rms_norm(h, attn_norm[l])
        if l < N_A:
            h = h + pool_mixer(hn, pool_w[l], pool_scale[l])
        else:
            j = l - N_A
            q = (hn @ wq[j]).reshape(B, S, N_HEADS, HEAD_DIM).transpose(0, 2, 1, 3)
            q = rope_partial(q, pos)
            o = moba_attention(q, kb, vb, k_mean)
            o = o.transpose(0, 2, 1, 3).reshape(B, S, D)
            h = h + o @ wo[j]
        hn = rms_norm(h, ffn_norm[l])
        if l % 2 == 0:
            i = l // 2
            h = h + swiglu(hn, ffn_w_gate[i], ffn_w_up[i], ffn_w_down[i])
        else:
            i = l // 2
            h = h + moe_swiglu(hn, router_w[i], moe_w_gate[i], moe_w_up[i], moe_w_down[i])
        if l == N_A - 1:
            kb, vb, k_mean = shared_kv(h, kv_norm, wk, wv, pos)
    return rms_norm(h, final_norm)
```

```python
import numpy as np
from contextlib import ExitStack
import concourse.bass as bass
import concourse.mybir as mybir
from concourse.bass_utils import run_bass_kernel_spmd

F32 = mybir.dt.float32
BF16 = mybir.dt.bfloat16
AF = mybir.ActivationFunctionType
ALU = mybir.AluOpType
AX = mybir.AxisListType

S = 4096
D = 1024
NCH = S // 128
NH = 8
HD = 128
BLK = 256
NBLK = S // BLK
F_DENSE = 2816
F_MOE = 3584
NE = 8
EPS = 1e-6
NEG = -30000.0
SCALE = HD ** -0.5


class Buf:
    __slots__ = ("name", "w", "r", "dsem", "dcnt", "excl")

    def __init__(self, name):
        self.name = name
        self.excl = False
        self.w = None
        self.r = {}
        self.dsem = {}
        self.dcnt = 0


class Builder:
    ENG = ("pe", "act", "dve", "pool", "sp")

    def __init__(self, nc):
        self.nc = nc
        self.esem = {e: nc.alloc_semaphore("es_" + e) for e in ("pe", "act", "dve", "pool")}
        self.ecnt = {e: 0 for e in self.esem}
        self.bar = nc.alloc_semaphore("bar")
        self.nbar = 0
        self.dpool = {"sp": [], "pool": []}
        self.nds = 0
        self._reset()

    def _reset(self):
        self.streams = {e: [] for e in self.ENG}
        self.waited = {e: {} for e in self.ENG}
        self.bufs = []

    def buf(self, name="b"):
        b = Buf(name)
        self.bufs.append(b)
        return b

    def bufs_n(self, n, name="b"):
        return [self.buf(f"{name}{i}") for i in range(n)]

    def _collect(self, eng, reads, writes):
        toks = {}

        def add(t):
            if t is None:
                return
            sem, val = t
            if toks.get(sem, (None, 0))[1] < val:
                toks[sem] = (sem, val)

        for b in reads:
            add(b.w)
            if b.excl:
                for t in b.r.values():
                    add(t)
        for b in writes:
            add(b.w)
            for t in b.r.values():
                add(t)
        out = []
        for sem, (_, val) in toks.items():
            if eng == "pe" and sem is self.esem["pe"]:
                continue
            if self.waited[eng].get(sem, 0) >= val:
                continue
            self.waited[eng][sem] = val
            out.append((sem, val))
        return out

    def _mark(self, tok, reads, writes):
        for b in writes:
            b.w = tok
            b.r = {}
        for b in reads:
            if b.r.get(tok[0], (None, 0))[1] < tok[1]:
                b.r[tok[0]] = tok

    def op(self, eng, fn, reads=(), writes=()):
        waits = self._collect(eng, reads, writes)
        self.ecnt[eng] += 1
        tok = (self.esem[eng], self.ecnt[eng])
        self.streams[eng].append((waits, fn, tok[0], 1))
        self._mark(tok, reads, writes)

    def dma(self, q, out, in_, owner, reads=(), writes=()):
        waits = self._collect(q, reads, writes)
        if q not in owner.dsem:
            if self.dpool[q]:
                owner.dsem[q] = list(self.dpool[q].pop())
            else:
                owner.dsem[q] = [self.nc.alloc_semaphore(f"ds{self.nds}"), 0]
                self.nds += 1
        owner.dsem[q][1] += 16
        tok = (owner.dsem[q][0], owner.dsem[q][1])
        self.streams[q].append((waits, (lambda e, o=out, i=in_: e.dma_start(out=o, in_=i)), tok[0], 16))
        self._mark(tok, reads, writes)

    def end_stage(self):
        waits = []
        for b in self.bufs:
            for (ds, dc) in b.dsem.values():
                waits.append((ds, dc))
        for e, s in self.esem.items():
            waits.append((s, self.ecnt[e]))
        self.nbar += 1
        nb = self.nbar
        bar = self.bar
        self.streams["sp"].append((waits, (lambda e: e.sem_inc(bar, 1)), None, 0))
        for e in self.ENG:
            self.streams[e].append(([(bar, nb)], None, None, 0))
        with self.nc.Block() as blk:
            for e, dec in (("pe", blk.tensor), ("act", blk.scalar), ("dve", blk.vector),
                           ("pool", blk.gpsimd), ("sp", blk.sync)):
                stream = self.streams[e]

                def body(eng, stream=stream):
                    for waits_, fn, sem, inc in stream:
                        for (s_, v_) in waits_:
                            eng.wait_ge(s_, v_)
                        if fn is not None:
                            ins = fn(eng)
                            if sem is not None:
                                ins.then_inc(sem, inc)

                dec(body)
        for b in self.bufs:
            for q, (ds, dc) in b.dsem.items():
                self.dpool[q].append((ds, dc))
        self._reset()


class Common:
    def __init__(self, B, es, nc, prefix):
        self.B = B
        self.nc = nc
        self.es = es
        self.prefix = prefix
        self.banks = [es.enter_context(nc.psum_tensor(f"{prefix}_bank{i}", [128, 512], F32)) for i in range(8)]
        self.bankb = B.bufs_n(8, "bank")
        for b in self.bankb:
            b.excl = True

    def sb(self, name, shape, dt):
        return self.es.enter_context(self.nc.sbuf_tensor(f"{self.prefix}_{name}", shape, dt))


def load_bcast(B, q, tile, vec_ap, buf):
    B.dma(q, tile[:], vec_ap.partition_broadcast(128), owner=buf, writes=[buf])


def emit_stats(B, C, srcs, src_bufs, stat, statbuf, junk):
    n = len(srcs)
    for i, (s, sbuf) in enumerate(zip(srcs, src_bufs)):
        B.op("act", lambda e, s=s, i=i: e.activation(out=junk[:], in_=s, func=AF.Square, accum_out=stat[:, 0, i:i + 1]),
             reads=[sbuf], writes=[statbuf])
    B.op("act", lambda e: e.activation(out=stat[:, 1, 0:n], in_=stat[:, 0, 0:n], func=AF.Sqrt, scale=1.0 / D, bias=EPS),
         reads=[statbuf], writes=[statbuf])
    B.op("dve", lambda e: e.reciprocal(out=stat[:, 2, 0:n], in_=stat[:, 1, 0:n]), reads=[statbuf], writes=[statbuf])


def emit_transpose8(B, C, src, src_buf, bank_i, ident, dst3, dst_bufs, evac_eng="act", ident_buf=None):
    pb = C.banks[bank_i][:].bitcast(BF16)
    bb = C.bankb[bank_i]

    def fn(e):
        ins = None
        for dc in range(8):
            ins = e.transpose(pb[:, dc * 128:(dc + 1) * 128], src[:, dc * 128:(dc + 1) * 128], ident[:])
        return ins

    B.op("pe", fn, reads=[src_buf] + ([ident_buf] if ident_buf is not None else []), writes=[bb])
    pv = pb.rearrange("p (c t) -> p c t", c=8)
    if evac_eng == "act":
        B.op("act", lambda e: e.copy(out=dst3, in_=pv), reads=[bb], writes=dst_bufs)
    else:
        B.op("dve", lambda e: e.tensor_copy(out=dst3, in_=pv), reads=[bb], writes=dst_bufs)


class FFN:
    def __init__(self, B, C, units, n_exp):
        self.B, self.C = B, C
        self.units = units
        self.n_exp = n_exp
        ufmax = max(n for _, n in units)
        self.ufmax = ufmax
        sb = C.sb
        self.NGU = 4
        self.wg = [sb(f"wg{i}", [128, 8, 256], BF16) for i in range(self.NGU)]
        self.wu = [sb(f"wu{i}", [128, 8, 256], BF16) for i in range(self.NGU)]
        self.wgb = B.bufs_n(self.NGU, "wg")
        self.wub = B.bufs_n(self.NGU, "wu")
        self.NWD = 2
        self.wd = [sb(f"wd{i}", [128, ufmax, 1024], BF16) for i in range(self.NWD)]
        self.wdb = B.bufs_n(self.NWD, "wd")
        self.actT = [sb(f"actT{i}", [128, ufmax, 1024], BF16) for i in range(2)]
        self.actb = [B.bufs_n(ufmax * 2, f"act{i}_") for i in range(2)]
        self.sg = [sb(f"sg{i}", [128, 512], BF16) for i in range(2)]
        self.sgb = B.bufs_n(2, "sg")
        self.gu_i = 0
        self.unit_i = 0
        self.pgu_i = 0
        self.pd_i = 0

    def run_tile(self, hnT, hnT_bufs, y, ybufs, wg_of, wu_of, wd_of, gates=None, gates_buf=None):
        B, C = self.B, self.C
        pending = None
        for e in range(self.n_exp):
            wg_d, wu_d, wd_d = wg_of(e), wu_of(e), wd_of(e)
            for (f0, n) in self.units:
                us = self.unit_i % 2
                ws = self.unit_i % self.NWD
                self.unit_i += 1
                for pr in range(n // 2):
                    gs = self.gu_i % self.NGU
                    self.gu_i += 1
                    c0 = (f0 + 2 * pr) * 128
                    B.dma("pool", self.wg[gs][:], wg_d[:, c0:c0 + 256].rearrange("(c p) f -> p c f", p=128),
                          owner=self.wgb[gs], writes=[self.wgb[gs]])
                    B.dma("pool", self.wu[gs][:], wu_d[:, c0:c0 + 256].rearrange("(c p) f -> p c f", p=128),
                          owner=self.wub[gs], writes=[self.wub[gs]])
                    for fl in range(2):
                        fci = 2 * pr + fl
                        for th in range(2):
                            k = self.pgu_i % 2
                            self.pgu_i += 1
                            pg, pu = C.banks[k], C.banks[2 + k]
                            pgb, pub = C.bankb[k], C.bankb[2 + k]

                            def mm(eng, ps=pg, w=self.wg[gs], fl=fl, th=th):
                                ins = None
                                for dc in range(8):
                                    ins = eng.matmul(ps[:], w[:, dc, fl * 128:(fl + 1) * 128],
                                                     hnT[:, dc, th * 512:(th + 1) * 512],
                                                     start=(dc == 0), stop=(dc == 7))
                                return ins

                            B.op("pe", mm, reads=[self.wgb[gs], hnT_bufs[th]], writes=[pgb])

                            def mm2(eng, ps=pu, w=self.wu[gs], fl=fl, th=th):
                                ins = None
                                for dc in range(8):
                                    ins = eng.matmul(ps[:], w[:, dc, fl * 128:(fl + 1) * 128],
                                                     hnT[:, dc, th * 512:(th + 1) * 512],
                                                     start=(dc == 0), stop=(dc == 7))
                                return ins

                            B.op("pe", mm2, reads=[self.wub[gs], hnT_bufs[th]], writes=[pub])
                            sgt, sgbuf = self.sg[k], self.sgb[k]
                            B.op("act", lambda eng, sgt=sgt, pg=pg: eng.activation(out=sgt[:], in_=pg[:], func=AF.Silu),
                                 reads=[pgb], writes=[sgbuf])
                            ab = self.actb[us][fci * 2 + th]
                            B.op("dve", lambda eng, sgt=sgt, pu=pu, a=self.actT[us], fci=fci, th=th:
                                 eng.tensor_tensor(out=a[:, fci, th * 512:(th + 1) * 512], in0=sgt[:], in1=pu[:], op=ALU.mult),
                                 reads=[sgbuf, pub], writes=[ab])
                B.dma("pool", self.wd[ws][:, 0:n, :],
                      wd_d[f0 * 128:(f0 + n) * 128, :].rearrange("(c p) d -> p c d", p=128),
                      owner=self.wdb[ws], writes=[self.wdb[ws]])
                if pending is not None:
                    self._down(*pending, y, ybufs, gates, gates_buf)
                pending = (us, ws, n, e)
        self._down(*pending, y, ybufs, gates, gates_buf)

    def _down(self, us, ws, n, e, y, ybufs, gates, gates_buf):
        B, C = self.B, self.C
        a, w = self.actT[us], self.wd[ws]
        for tc in range(8):
            th = tc // 4
            k = self.pd_i % 2
            self.pd_i += 1
            for dh in range(2):
                bi = 4 + 2 * k + dh
                ps, psb = C.banks[bi], C.bankb[bi]

                def mm(eng, ps=ps, tc=tc, dh=dh):
                    ins = None
                    for fci in range(n):
                        ins = eng.matmul(ps[:], a[:, fci, tc * 128:(tc + 1) * 128], w[:, fci, dh * 512:(dh + 1) * 512],
                                         start=(fci == 0), stop=(fci == n - 1))
                    return ins

                B.op("pe", mm, reads=[self.actb[us][fci * 2 + th] for fci in range(n)] + [self.wdb[ws]], writes=[psb])
                ysl = y[:, tc, dh * 512:(dh + 1) * 512]
                if gates is None:
                    B.op("dve", lambda eng, ysl=ysl, ps=ps: eng.tensor_tensor(out=ysl, in0=ps[:], in1=ysl, op=ALU.add),
                         reads=[psb, ybufs[tc]], writes=[ybufs[tc]])
                else:
                    B.op("dve", lambda eng, ysl=ysl, ps=ps, tc=tc, e=e: eng.scalar_tensor_tensor(
                        out=ysl, in0=ps[:], scalar=gates[:, tc, e:e + 1], in1=ysl, op0=ALU.mult, op1=ALU.add),
                        reads=[psb, ybufs[tc], gates_buf], writes=[ybufs[tc]])


def stage_A1(nc, B, T):
    with ExitStack() as es:
        C = Common(B, es, nc, "a1")
        sb = C.sb
        g1 = sb("g1", [128, D], F32)
        g2 = sb("g2", [128, D], F32)
        psc = sb("psc", [128, D], F32)
        band = sb("band", [128, 12, 128], BF16)
        ident = sb("ident", [128, 128], BF16)
        wp = sb("wp", [128, 8, 256], BF16)
        y = sb("y", [128, 8, D], F32)
        hn = [sb(f"hn{i}", [128, D], BF16) for i in range(3)]
        hn2 = [sb(f"hn2_{i}", [128, D], BF16) for i in range(2)]
        hnT = sb("hnT", [128, 8, 1024], BF16)
        pooledT = [sb(f"pooledT{i}", [128, 8, 128], BF16) for i in range(2)]
        junk = sb("junk", [128, D], BF16)
        stat = [sb(f"stat{i}", [128, 3, 8], F32) for i in range(2)]
        g1b, g2b, pscb, bandb, identb, wpb = (B.buf(n) for n in ("g1", "g2", "psc", "band", "ident", "wp"))
        ybufs = B.bufs_n(8, "y")
        hnb = B.bufs_n(3, "hn")
        hn2b = B.bufs_n(2, "hn2")
        hnTb = B.bufs_n(2, "hnT")
        pTb = B.bufs_n(2, "pooledT")
        statb = B.bufs_n(2, "stat")
        ffn = FFN(B, C, [(0, 6), (6, 6), (12, 6), (18, 4)], 1)

        load_bcast(B, "sp", g1, T["attn_norm"][0, :], g1b)
        load_bcast(B, "sp", g2, T["ffn_norm"][0, :], g2b)
        load_bcast(B, "sp", psc, T["pool_scale"][0, :], pscb)
        B.dma("pool", band[:], T["c_band"], owner=bandb, writes=[bandb])
        B.dma("pool", ident[:], T["c_ident"], owner=identb, writes=[identb])
        wp32 = y[:, 0:2, :].rearrange("p a (b d) -> p (a b) d", d=256)
        B.dma("sp", wp32, T["pool_w"][0].rearrange("g (k p) d -> p (g k) d", p=128), owner=ybufs[0],
              writes=[ybufs[0], ybufs[1]])
        for g in range(4):
            B.op("dve", lambda e, g=g: e.tensor_tensor(
                out=wp[:, 2 * g:2 * g + 2, :], in0=wp32[:, 2 * g:2 * g + 2, :],
                in1=psc[:, g * 256:(g + 1) * 256].rearrange("p (o d) -> p o d", o=1).broadcast_to([128, 2, 256]),
                op=ALU.mult), reads=[ybufs[0], ybufs[1], pscb], writes=[wpb])

        gchunk = 0
        for tt in range(4):
            t0 = tt * 1024
            for c in range(8):
                B.dma("sp", y[:, c, :], T["x"][t0 + c * 128:t0 + (c + 1) * 128, :], owner=ybufs[c], writes=[ybufs[c]])
            st, stb = stat[0], statb[0]
            emit_stats(B, C, [y[:, c, :] for c in range(8)], ybufs, st, stb, junk)
            def a1_front(c, g, st=st):
                hs = g % 3
                hp = (g - 1) % 3
                pb0 = 4 if g % 2 == 0 else 0
                B.op("dve", lambda e, c=c, hs=hs, st=st: e.scalar_tensor_tensor(
                    out=hn[hs][:], in0=y[:, c, :], scalar=st[:, 2, c:c + 1], in1=g1[:], op0=ALU.mult, op1=ALU.mult),
                    reads=[ybufs[c], stb, g1b], writes=[hnb[hs]])
                first = (g == 0)

                def pool_mm(e, hs=hs, hp=hp, first=first, pb0=pb0):
                    ins = None
                    for dc in range(8):
                        w = dc // 2
                        out = C.banks[pb0 + dc // 4][:, (dc % 4) * 128:(dc % 4 + 1) * 128]
                        if first:
                            ins = e.matmul(out, hn[hs][:, dc * 128:(dc + 1) * 128], band[:, 8 + w, :], start=True, stop=True)
                        else:
                            e.matmul(out, hn[hp][:, dc * 128:(dc + 1) * 128], band[:, 4 + w, :], start=True, stop=False)
                            ins = e.matmul(out, hn[hs][:, dc * 128:(dc + 1) * 128], band[:, w, :], start=False, stop=True)
                    return ins

                B.op("pe", pool_mm, reads=[hnb[hs], bandb] + ([] if first else [hnb[hp]]), writes=[C.bankb[pb0], C.bankb[pb0 + 1]])

            def a1_back(c, g):
                pb0 = 4 if g % 2 == 0 else 0
                ps_ = g % 2
                for hb in range(2):
                    B.op("act", lambda e, ps_=ps_, hb=hb, pb0=pb0: e.copy(
                        out=pooledT[ps_][:, hb * 4:(hb + 1) * 4, :],
                        in_=C.banks[pb0 + hb][:].rearrange("p (c t) -> p c t", c=4)),
                        reads=[C.bankb[pb0 + hb]], writes=[pTb[ps_]])

                def y_mm(e, ps_=ps_):
                    ins = None
                    for g_ in range(4):
                        out = C.banks[6 + g_ // 2][:, (g_ % 2) * 256:(g_ % 2 + 1) * 256]
                        for k in range(2):
                            ins = e.matmul(out, pooledT[ps_][:, 2 * g_ + k, :], wp[:, 2 * g_ + k, :], start=(k == 0), stop=(k == 1))
                    return ins

                B.op("pe", y_mm, reads=[pTb[ps_], wpb], writes=[C.bankb[6], C.bankb[7]])
                for hb in range(2):
                    ysl = y[:, c, hb * 512:(hb + 1) * 512]
                    B.op("dve", lambda e, ysl=ysl, hb=hb: e.tensor_tensor(out=ysl, in0=C.banks[6 + hb][:], in1=ysl, op=ALU.add),
                         reads=[C.bankb[6 + hb], ybufs[c]], writes=[ybufs[c]])

            a1_front(0, gchunk)
            for c in range(1, 8):
                a1_front(c, gchunk + c)
                a1_back(c - 1, gchunk + c - 1)
            a1_back(7, gchunk + 7)
            gchunk += 8
            if "h1" in T:
                for c in range(8):
                    B.dma("sp", T["h1"][t0 + c * 128:t0 + (c + 1) * 128, :], y[:, c, :], owner=ybufs[c], reads=[ybufs[c]])
            st, stb = stat[1], statb[1]
            emit_stats(B, C, [y[:, c, :] for c in range(8)], ybufs, st, stb, junk)
            for c in range(8):
                hs = c % 2
                B.op("dve", lambda e, c=c, hs=hs, st=st: e.scalar_tensor_tensor(
                    out=hn2[hs][:], in0=y[:, c, :], scalar=st[:, 2, c:c + 1], in1=g2[:], op0=ALU.mult, op1=ALU.mult),
                    reads=[ybufs[c], stb, g2b], writes=[hn2b[hs]])
                emit_transpose8(B, C, hn2[hs], hn2b[hs], 0 + (c % 2), ident,
                                hnT[:, :, c * 128:(c + 1) * 128], [hnTb[c // 4]], ident_buf=identb)
            ffn.run_tile(hnT, hnTb, y, ybufs,
                         lambda e: T["ffn_w_gate"][0], lambda e: T["ffn_w_up"][0], lambda e: T["ffn_w_down"][0])
            for c in range(8):
                B.dma("sp", T["h2"][t0 + c * 128:t0 + (c + 1) * 128, :], y[:, c, :], owner=ybufs[c], reads=[ybufs[c]])
        B.end_stage()


def make_consts():
    band = np.zeros((128, 12, 128), np.float32)
    s = np.arange(128)[:, None]
    t = np.arange(128)[None, :]
    for wi, w in enumerate((2, 4, 8, 16)):
        dlt = t - s
        band[:, wi, :] = np.where((dlt >= 0) & (dlt < w), 1.0 / w, 0.0) - (dlt == 0)
        band[:, 4 + wi, :] = np.where((t + 128 - s) < w, 1.0 / w, 0.0)
        cnt = np.minimum(t + 1, w).astype(np.float32)
        band[:, 8 + wi, :] = np.where((dlt >= 0) & (dlt < w), 1.0 / cnt, 0.0) - (dlt == 0)
    ident = np.eye(128, dtype=np.float32)
    half = 16
    inv = (np.float32(500000.0) ** (-np.arange(half, dtype=np.float32) * np.float32(2.0) / np.float32(32))).astype(np.float32)
    ang = (np.arange(S, dtype=np.float32)[:, None] * inv[None, :]).astype(np.float32)
    cos8, sin8 = np.cos(ang.astype(np.float64)).astype(np.float32), np.sin(ang.astype(np.float64)).astype(np.float32)
    q = np.arange(128)[:, None]
    k = np.arange(128)[None, :]
    tri = np.where(k <= q, 0.0, NEG).astype(np.float32)
    bb = np.arange(NBLK)[:, None]
    nn = np.arange(NBLK)[None, :]
    past = np.where(nn < bb, 0.0, -1e30).astype(np.float32).reshape(-1)
    cos8 = np.ascontiguousarray(cos8.reshape(NCH, 128, 16).transpose(1, 0, 2))
    sin8 = np.ascontiguousarray(sin8.reshape(NCH, 128, 16).transpose(1, 0, 2))
    return {"c_band": band, "c_ident": ident, "c_cos": cos8, "c_sin": sin8, "c_tri": tri, "c_past": past}


def stage_C(nc, B, T, src="h3"):
    with ExitStack() as es:
        C = Common(B, es, nc, "c")
        sb = C.sb
        g2 = sb("g2", [128, D], F32)
        gf = sb("gf", [128, D], F32)
        ident32 = sb("ident32", [128, 128], F32)
        wr = sb("wr", [128, 8, NE], F32)
        y = sb("y", [128, 8, D], F32)
        hnf = [sb(f"hnf{i}", [128, D], F32) for i in range(2)]
        hnT = sb("hnT", [128, 8, 1024], BF16)
        hnT32s = [sb(f"hnT32_{i}", [128, 8, 128], F32) for i in range(2)]
        gates = sb("gates", [128, 8, NE], F32)
        rt = [sb(f"rt{i}", [128, 6, NE], F32) for i in range(2)]
        junk = sb("junk", [128, D], BF16)
        stat = [sb(f"stat{i}", [128, 3, 8], F32) for i in range(2)]
        g2b, gfb, id32b, wrb, gatesb = (B.buf(n) for n in ("g2", "gf", "id32", "wr", "gates"))
        hnT32bs = B.bufs_n(2, "hnT32")
        ybufs = B.bufs_n(8, "y")
        hnfb = B.bufs_n(2, "hnf")
        hnTb = B.bufs_n(2, "hnT")
        rtb = B.bufs_n(2, "rt")
        statb = B.bufs_n(2, "stat")
        ffn = FFN(B, C, [(0, 8), (8, 8), (16, 8), (24, 4)], NE)

        load_bcast(B, "sp", g2, T["ffn_norm"][1, :], g2b)
        load_bcast(B, "sp", gf, T["final_norm"], gfb)
        B.dma("sp", ident32[:], T["c_ident"], owner=id32b, writes=[id32b])
        B.dma("sp", wr[:], T["router_w"][0].rearrange("(c p) e -> p c e", p=128), owner=wrb, writes=[wrb])

        for tt in range(4):
            t0 = tt * 1024
            if tt == 0:
                for c in range(8):
                    B.dma("sp", y[:, c, :], T[src][t0 + c * 128:t0 + (c + 1) * 128, :], owner=ybufs[c], writes=[ybufs[c]])
            st, stb = stat[0], statb[0]
            emit_stats(B, C, [y[:, c, :] for c in range(8)], ybufs, st, stb, junk)
            def c_front(c, st=st):
                hs = c % 2
                hnT32, hnT32b = hnT32s[c % 2], hnT32bs[c % 2]
                B.op("dve", lambda e, c=c, hs=hs, st=st: e.scalar_tensor_tensor(
                    out=hnf[hs][:], in0=y[:, c, :], scalar=st[:, 2, c:c + 1], in1=g2[:], op0=ALU.mult, op1=ALU.mult),
                    reads=[ybufs[c], stb, g2b], writes=[hnfb[hs]])

                def tr(e, hs=hs):
                    ins = None
                    for dc in range(8):
                        ins = e.transpose(C.banks[4 + dc // 4][:, (dc % 4) * 128:(dc % 4 + 1) * 128],
                                          hnf[hs][:, dc * 128:(dc + 1) * 128], ident32[:])
                    return ins

                B.op("pe", tr, reads=[hnfb[hs], id32b], writes=[C.bankb[4], C.bankb[5]])
                for hb in range(2):
                    pv = C.banks[4 + hb][:].rearrange("p (c t) -> p c t", c=4)
                    B.op("act", lambda e, pv=pv, hb=hb, c=c: e.copy(out=hnT[:, hb * 4:(hb + 1) * 4, c * 128:(c + 1) * 128], in_=pv),
                         reads=[C.bankb[4 + hb]], writes=[hnTb[c // 4]])
                    B.op("dve", lambda e, pv=pv, hb=hb: e.tensor_copy(out=hnT32[:, hb * 4:(hb + 1) * 4, :], in_=pv),
                         reads=[C.bankb[4 + hb]], writes=[hnT32b])

            def c_back(c):
                hnT32, hnT32b = hnT32s[c % 2], hnT32bs[c % 2]
                def lg_mm(e):
                    ins = None
                    for dc in range(8):
                        ins = e.matmul(C.banks[6][:, 0:NE], hnT32[:, dc, :], wr[:, dc, :], start=(dc == 0), stop=(dc == 7))
                    return ins

                B.op("pe", lg_mm, reads=[hnT32b, wrb], writes=[C.bankb[6]])
                r, rb = rt[c % 2], rtb[c % 2]
                B.op("dve", lambda e, r=r: e.tensor_copy(out=r[:, 0, :], in_=C.banks[6][:, 0:NE]), reads=[C.bankb[6]], writes=[rb])
                B.op("dve", lambda e, r=r: e.max(out=r[:, 1, :], in_=r[:, 0, :]), reads=[rb], writes=[rb])
                B.op("dve", lambda e, r=r: e.tensor_scalar(out=r[:, 2, :], in0=r[:, 0, :], scalar1=r[:, 1, 0:1], scalar2=None,
                                                           op0=ALU.subtract), reads=[rb], writes=[rb])
                B.op("dve", lambda e, r=r: e.tensor_scalar(out=r[:, 3, :], in0=r[:, 0, :], scalar1=r[:, 1, 1:2], scalar2=None,
                                                           op0=ALU.is_ge), reads=[rb], writes=[rb])
                B.op("act", lambda e, r=r: e.activation(out=r[:, 2, :], in_=r[:, 2, :], func=AF.Exp), reads=[rb], writes=[rb])
                B.op("dve", lambda e, r=r: e.tensor_tensor(out=r[:, 4, :], in0=r[:, 2, :], in1=r[:, 3, :], op=ALU.mult),
                     reads=[rb], writes=[rb])
                B.op("dve", lambda e, r=r: e.tensor_reduce(out=r[:, 5, 0:1], in_=r[:, 4, :], axis=AX.X, op=ALU.add),
                     reads=[rb], writes=[rb])
                B.op("dve", lambda e, r=r: e.reciprocal(out=r[:, 5, 1:2], in_=r[:, 5, 0:1]), reads=[rb], writes=[rb])
                B.op("dve", lambda e, r=r, c=c: e.tensor_scalar(out=gates[:, c, :], in0=r[:, 4, :], scalar1=r[:, 5, 1:2], scalar2=None,
                                                                op0=ALU.mult), reads=[rb], writes=[gatesb])

            c_front(0)
            for c in range(1, 8):
                c_front(c)
                c_back(c - 1)
            c_back(7)
            if "gates" in T:
                B.dma("sp", T["gates"][t0:t0 + 1024, :].rearrange("(c p) e -> p c e", p=128), gates[:], owner=gatesb, reads=[gatesb])
            ffn.run_tile(hnT, hnTb, y, ybufs,
                         lambda e: T["moe_w_gate"][0, e], lambda e: T["moe_w_up"][0, e], lambda e: T["moe_w_down"][0, e],
                         gates=gates, gates_buf=gatesb)
            st, stb = stat[1], statb[1]
            emit_stats(B, C, [y[:, c, :] for c in range(8)], ybufs, st, stb, junk)
            for c in range(8):
                hs = c % 2
                B.op("dve", lambda e, c=c, hs=hs, st=st: e.scalar_tensor_tensor(
                    out=hnf[hs][:], in0=y[:, c, :], scalar=st[:, 2, c:c + 1], in1=gf[:], op0=ALU.mult, op1=ALU.mult),
                    reads=[ybufs[c], stb, gfb], writes=[hnfb[hs]])
                B.dma("sp", T["out"][t0 + c * 128:t0 + (c + 1) * 128, :], hnf[hs][:], owner=hnfb[hs], reads=[hnfb[hs]])
                if tt < 3:
                    t1 = t0 + 1024
                    B.dma("sp", y[:, c, :], T[src][t1 + c * 128:t1 + (c + 1) * 128, :], owner=ybufs[c], writes=[ybufs[c]])
        B.end_stage()


def emit_rope(B, src_bank, src_bankb, dst3, dst_buf, cos_c, sin_c, tmp, tmpb, ropeb):
    pv = src_bank[:].rearrange("p (h d) -> p h d", h=4)
    x1, x2 = pv[:, :, 0:16], pv[:, :, 16:32]
    cb = cos_c.rearrange("p (o k) -> p o k", o=1).broadcast_to([128, 4, 16])
    sbb = sin_c.rearrange("p (o k) -> p o k", o=1).broadcast_to([128, 4, 16])
    for i, (a, b_) in enumerate(((x1, cb), (x2, sbb), (x2, cb), (x1, sbb))):
        B.op("dve", lambda e, a=a, b_=b_, i=i: e.tensor_tensor(out=tmp[:, i, :, :], in0=a, in1=b_, op=ALU.mult),
             reads=[src_bankb, ropeb], writes=[tmpb])
    B.op("dve", lambda e: e.tensor_tensor(out=dst3[:, :, 0:16], in0=tmp[:, 0, :, :], in1=tmp[:, 1, :, :], op=ALU.subtract),
         reads=[tmpb], writes=[dst_buf])
    B.op("dve", lambda e: e.tensor_tensor(out=dst3[:, :, 16:32], in0=tmp[:, 2, :, :], in1=tmp[:, 3, :, :], op=ALU.add),
         reads=[tmpb], writes=[dst_buf])


def emit_rope_sb(B, dst3, dst_buf, cos_c, sin_c, tmp, tmpb, ropeb):
    x1, x2 = dst3[:, :, 0:16], dst3[:, :, 16:32]
    cb = cos_c.rearrange("p (o k) -> p o k", o=1).broadcast_to([128, NH, 16])
    sbb = sin_c.rearrange("p (o k) -> p o k", o=1).broadcast_to([128, NH, 16])
    for i, (a, b_) in enumerate(((x1, cb), (x2, sbb), (x2, cb), (x1, sbb))):
        B.op("dve", lambda e, a=a, b_=b_, i=i: e.tensor_tensor(out=tmp[:, i, :, :], in0=a, in1=b_, op=ALU.mult),
             reads=[dst_buf, ropeb], writes=[tmpb])
    B.op("dve", lambda e: e.tensor_tensor(out=x1, in0=tmp[:, 0, :, :], in1=tmp[:, 1, :, :], op=ALU.subtract),
         reads=[tmpb], writes=[dst_buf])
    B.op("dve", lambda e: e.tensor_tensor(out=x2, in0=tmp[:, 2, :, :], in1=tmp[:, 3, :, :], op=ALU.add),
         reads=[tmpb], writes=[dst_buf])


def stage_A2(nc, B, T):
    with ExitStack() as es:
        C = Common(B, es, nc, "a2")
        sb = C.sb
        gk = sb("gk", [128, D], F32)
        ident = sb("ident", [128, 128], BF16)
        wk = sb("wk", [128, 8, D], BF16)
        wv = sb("wv", [128, 8, D], BF16)
        cos = sb("cos", [128, NCH, 16], F32)
        sin = sb("sin", [128, NCH, 16], F32)
        ones = sb("ones", [128, 1], BF16)
        y = sb("y", [128, 8, D], F32)
        hn = [sb(f"hn{i}", [128, D], BF16) for i in range(2)]
        hnTc = [sb(f"hnTc{i}", [128, 8, 128], BF16) for i in range(2)]
        Kb = [sb(f"Kb{i}", [128, D], BF16) for i in range(2)]
        Vb = [sb(f"Vb{i}", [128, D], BF16) for i in range(2)]
        KT4 = [sb(f"KT4_{i}", [128, 8, 512], BF16) for i in range(2)]
        rtmp = [sb(f"rtmp{i}", [128, 4, NH, 16], F32) for i in range(2)]
        sq = sb("sq", [128, 512], F32)
        kn2 = sb("kn2", [128, 2, 8], F32)
        kmT = sb("kmT", [128, 8, NBLK], F32)
        junk = sb("junk", [128, D], BF16)
        stat = sb("stat", [128, 3, 8], F32)
        gkb, identb, wkb, wvb, ropeb, onesb, kn2b, kmTb, statb, sqb = (
            B.buf(n) for n in ("gk", "ident", "wk", "wv", "rope", "ones", "kn2", "kmT", "stat", "sq"))
        ybufs = B.bufs_n(8, "y")
        hnb = B.bufs_n(2, "hn")
        hnTcb = B.bufs_n(2, "hnTc")
        Kbb = B.bufs_n(2, "Kb")
        Vbb = B.bufs_n(2, "Vb")
        KT4b = B.bufs_n(2, "KT4")
        rtmpb = B.bufs_n(2, "rtmp")

        load_bcast(B, "sp", gk, T["kv_norm"], gkb)
        B.dma("pool", ident[:], T["c_ident"], owner=identb, writes=[identb])
        B.dma("pool", wk[:], T["wk"].rearrange("(c p) n -> p c n", p=128), owner=wkb, writes=[wkb])
        B.dma("pool", wv[:], T["wv"].rearrange("(c p) n -> p c n", p=128), owner=wvb, writes=[wvb])
        B.dma("sp", cos[:], T["c_cos"], owner=ropeb, writes=[ropeb])
        B.dma("sp", sin[:], T["c_sin"], owner=ropeb, writes=[ropeb])
        B.op("dve", lambda e: e.memset(ones[:], 1.0 / BLK), writes=[onesb])
        B.op("dve", lambda e: e.memset(kn2[:], 0.0), writes=[kn2b])

        for tt in range(4):
            t0 = tt * 1024
            for c in range(8):
                B.dma("sp", y[:, c, :], T["h2"][t0 + c * 128:t0 + (c + 1) * 128, :], owner=ybufs[c], writes=[ybufs[c]])
            emit_stats(B, C, [y[:, c, :] for c in range(8)], ybufs, stat, statb, junk)
            def a2_front(c):
                gc = tt * 8 + c
                s2 = gc % 2
                kbk = (1, 2) if gc % 2 == 0 else (7, 6)
                B.op("dve", lambda e, c=c, s2=s2: e.scalar_tensor_tensor(
                    out=hn[s2][:], in0=y[:, c, :], scalar=stat[:, 2, c:c + 1], in1=gk[:], op0=ALU.mult, op1=ALU.mult),
                    reads=[ybufs[c], statb, gkb], writes=[hnb[s2]])
                emit_transpose8(B, C, hn[s2], hnb[s2], 0, ident, hnTc[s2][:, :, :], [hnTcb[s2]], ident_buf=identb)
                for (w, wb, bks) in ((wk, wkb, kbk),):
                    for half in range(2):
                        def mm(e, w=w, half=half, bk=bks[half], s2=s2):
                            ins = None
                            for dc in range(8):
                                ins = e.matmul(C.banks[bk][:], hnTc[s2][:, dc, :], w[:, dc, half * 512:(half + 1) * 512],
                                               start=(dc == 0), stop=(dc == 7))
                            return ins
                        B.op("pe", mm, reads=[hnTcb[s2], wb], writes=[C.bankb[bks[half]]])

            def a2_back(c):
                gc = tt * 8 + c
                s2 = gc % 2
                kbk = (1, 2) if gc % 2 == 0 else (7, 6)
                for half in range(2):
                    def mmv(e, half=half, s2=s2):
                        ins = None
                        for dc in range(8):
                            ins = e.matmul(C.banks[3 + half][:], hnTc[s2][:, dc, :], wv[:, dc, half * 512:(half + 1) * 512],
                                           start=(dc == 0), stop=(dc == 7))
                        return ins
                    B.op("pe", mmv, reads=[hnTcb[s2], wvb], writes=[C.bankb[3 + half]])
                for half in range(2):
                    B.op("act", lambda e, half=half, s2=s2: e.copy(out=Vb[s2][:, half * 512:(half + 1) * 512], in_=C.banks[3 + half][:]),
                         reads=[C.bankb[3 + half]], writes=[Vbb[s2]])
                B.dma("sp", T["Vp"][:, :, gc, :].rearrange("h p d -> p h d"), Vb[s2][:].rearrange("p (h d) -> p h d", h=8),
                      owner=Vbb[s2], reads=[Vbb[s2]])
                for half in range(2):
                    kb_ = kbk[half]
                    B.op("act", lambda e, half=half, s2=s2, kb_=kb_: e.copy(out=Kb[s2][:, half * 512:(half + 1) * 512], in_=C.banks[kb_][:]),
                         reads=[C.bankb[kb_]], writes=[Kbb[s2]])
                    B.op("act", lambda e, half=half, kb_=kb_: e.activation(out=sq[:], in_=C.banks[kb_][:], func=AF.Square),
                         reads=[C.bankb[kb_]], writes=[sqb])
                    B.op("dve", lambda e, half=half: e.tensor_reduce(out=kn2[:, 0, half * 4:(half + 1) * 4],
                                                                     in_=sq[:].rearrange("p (h d) -> p h d", h=4), axis=AX.X, op=ALU.add),
                         reads=[sqb], writes=[kn2b])
                emit_rope_sb(B, Kb[s2][:].rearrange("p (h d) -> p h d", h=8), Kbb[s2],
                             cos[:, gc, :], sin[:, gc, :], rtmp[s2], rtmpb[s2], ropeb)
                B.op("dve", lambda e: e.tensor_tensor(out=kn2[:, 1, :], in0=kn2[:, 1, :], in1=kn2[:, 0, :], op=ALU.max),
                     reads=[kn2b], writes=[kn2b])
                k4 = (gc // 4) % 2
                j4 = gc % 4
                emit_transpose8(B, C, Kb[s2], Kbb[s2], 5, ident, KT4[k4][:, :, j4 * 128:(j4 + 1) * 128], [KT4b[k4]], evac_eng="act", ident_buf=identb)
                if j4 == 3:
                    tq = (gc // 4) * 512
                    B.dma("sp", T["KT"][:, :, tq:tq + 512].rearrange("h p t -> p h t"), KT4[k4][:], owner=KT4b[k4], reads=[KT4b[k4]])
                if gc % 2 == 1:
                    blk = gc // 2
                    sp_ = (gc - 1) % 2

                    def km(e, sp_=sp_, s2=s2):
                        ins = None
                        for h in range(8):
                            e.matmul(C.banks[0][:, h:h + 1], Kb[sp_][:, h * 128:(h + 1) * 128], ones[:], start=True, stop=False)
                            ins = e.matmul(C.banks[0][:, h:h + 1], Kb[s2][:, h * 128:(h + 1) * 128], ones[:], start=False, stop=True)
                        return ins

                    B.op("pe", km, reads=[Kbb[sp_], Kbb[s2], onesb], writes=[C.bankb[0]])
                    B.op("dve", lambda e, blk=blk: e.tensor_copy(out=kmT[:, :, blk], in_=C.banks[0][:, 0:8]),
                         reads=[C.bankb[0]], writes=[kmTb])

            a2_front(0)
            for c in range(1, 8):
                a2_front(c)
                a2_back(c - 1)
            a2_back(7)
        B.dma("sp", T["kmT"], kmT[:].rearrange("p h n -> p (h n)"), owner=kmTb, reads=[kmTb])
        B.dma("sp", T["kn2"], kn2[:, 1, :], owner=kn2b, reads=[kn2b])
        B.end_stage()


def stage_B(nc, B, T):
    with ExitStack() as es:
        C = Common(B, es, nc, "b")
        sb = C.sb
        g1 = sb("g1", [128, D], F32)
        ident = sb("ident", [128, 128], BF16)
        wq = sb("wq", [128, 8, D], BF16)
        wo = sb("wo", [128, 8, D], BF16)
        cos = sb("cos", [128, NCH, 16], F32)
        sin = sb("sin", [128, NCH, 16], F32)
        tri = sb("tri", [128, 128], F32)
        past = sb("past", [128, NBLK, NBLK], F32)
        kmT32 = sb("kmT32", [128, NH * NBLK], F32)
        kmT = sb("kmT", [128, NH, NBLK], BF16)
        kn2all = sb("kn2all", [128, 128 * NH], F32)
        kmax2 = sb("kmax2", [128, NH], F32)
        y = sb("y", [128, 4, D], F32)
        hn = [sb(f"hn{i}", [128, D], BF16) for i in range(2)]
        hnTc = [sb(f"hnTc{i}", [128, 8, 128], BF16) for i in range(2)]
        Qb = [sb(f"Qb{i}", [128, D], BF16) for i in range(2)]
        QT = sb("QT", [128, NH, 512], BF16)
        rtmp = [sb(f"rtmp{i}", [128, 4, NH, 16], F32) for i in range(2)]
        sq = sb("sq", [128, 512], F32)
        qn = sb("qn", [128, 4, 3, NH], F32)
        KTh = [sb(f"KTh{i}", [128, S], BF16) for i in range(2)]
        Vh = [sb(f"Vh{i}", [128, NCH, HD + 1], BF16) for i in range(2)]
        P = [sb(f"P{i}", [128, S], BF16) for i in range(2)]
        PT = [sb(f"PT{i}", [128, NCH, 128], BF16) for i in range(2)]
        dg = [sb(f"dg{i}", [128, 128], F32) for i in range(2)]
        gt = [sb(f"gt{i}", [128, 4, NBLK], F32) for i in range(3)]
        rs = [sb(f"rs{i}", [128, 24], F32) for i in range(2)]
        Osb = [sb(f"Osb{i}", [128, D], BF16) for i in range(4)]
        OT = [sb(f"OT{i}", [128, 8, 128], BF16) for i in range(2)]
        junk = sb("junk", [128, D], BF16)
        stat = sb("stat", [128, 3, 8], F32)
        (g1b, identb, wqb, wob, ropeb, trib, pastb, kmTb, kmaxb, QTb_unused, statb, sqb) = (
            B.buf(n) for n in ("g1", "ident", "wq", "wo", "rope", "tri", "past", "kmT", "kmax", "x", "stat", "sq"))
        ybufs = B.bufs_n(4, "y")
        hnb = B.bufs_n(2, "hn")
        hnTcb = B.bufs_n(2, "hnTc")
        Qbb = B.bufs_n(2, "Qb")
        QTb = B.bufs_n(4, "QT")
        rtmpb = B.bufs_n(2, "rtmp")
        qnb = B.bufs_n(4, "qn")
        KThb = B.bufs_n(2, "KTh")
        Vhb = B.bufs_n(2, "Vh")
        Pb = B.bufs_n(2, "P")
        PTb = [B.bufs_n(4, f"PT{i}_") for i in range(2)]
        dgb = B.bufs_n(2, "dg")
        gtb = B.bufs_n(3, "gt")
        rsb = B.bufs_n(2, "rs")
        onesb = B.buf("vones")
        Osbb = B.bufs_n(4, "Osb")
        OTb = B.bufs_n(2, "OT")

        load_bcast(B, "sp", g1, T["attn_norm"][1, :], g1b)
        B.dma("pool", ident[:], T["c_ident"], owner=identb, writes=[identb])
        B.dma("pool", wq[:], T["wq"][0].rearrange("(c p) n -> p c n", p=128), owner=wqb, writes=[wqb])
        B.dma("pool", wo[:], T["wo"][0].rearrange("(c p) n -> p c n", p=128), owner=wob, writes=[wob])
        B.dma("sp", cos[:], T["c_cos"], owner=ropeb, writes=[ropeb])
        B.dma("sp", sin[:], T["c_sin"], owner=ropeb, writes=[ropeb])
        B.dma("sp", tri[:], T["c_tri"], owner=trib, writes=[trib])
        B.dma("sp", past[:].rearrange("p a b -> p (a b)"), T["c_past"].partition_broadcast(128), owner=pastb, writes=[pastb])
        B.dma("sp", kmT32[:], T["kmT"], owner=kmTb, writes=[kmTb])
        B.op("dve", lambda e: e.tensor_copy(out=kmT[:].rearrange("p h n -> p (h n)"), in_=kmT32[:]), reads=[kmTb], writes=[kmTb])
        B.dma("sp", kn2all[:], T["kn2"].rearrange("p h -> (p h)").partition_broadcast(128), owner=kmaxb, writes=[kmaxb])
        B.op("dve", lambda e: e.tensor_reduce(out=kmax2[:], in_=kn2all[:].rearrange("q (p h) -> q h p", h=NH), axis=AX.X, op=ALU.max),
             reads=[kmaxb], writes=[kmaxb])

        self_sb = [0]
        self_tb = [0]
        gt_i = 0
        for i in range(2):
            B.op("dve", lambda e, i=i: e.memset(Vh[i][:, :, HD:HD + 1], 1.0), writes=[onesb])
        hq_i = 0
        at_i = 0
        for tt in range(8):
            t0 = tt * 512
            nk_t = (tt + 1) * 4
            if tt == 0:
                for c in range(4):
                    B.dma("sp", y[:, c, :], T["h2"][t0 + c * 128:t0 + (c + 1) * 128, :], owner=ybufs[c], writes=[ybufs[c]])
                emit_stats(B, C, [y[:, c, :] for c in range(4)], ybufs, stat, statb, junk)
            def b_front(c):
                gc = tt * 4 + c
                s2 = gc % 2
                qbk = (1, 2) if c % 2 == 0 else (5, 6)
                B.op("dve", lambda e, c=c, s2=s2: e.scalar_tensor_tensor(
                    out=hn[s2][:], in0=y[:, c, :], scalar=stat[:, 2, c:c + 1], in1=g1[:], op0=ALU.mult, op1=ALU.mult),
                    reads=[ybufs[c], statb, g1b], writes=[hnb[s2]])
                emit_transpose8(B, C, hn[s2], hnb[s2], 0, ident, hnTc[s2][:, :, :], [hnTcb[s2]], ident_buf=identb)
                for half in range(2):
                    def mm(e, half=half, s2=s2, bk=qbk[half]):
                        ins = None
                        for dc in range(8):
                            ins = e.matmul(C.banks[bk][:], hnTc[s2][:, dc, :], wq[:, dc, half * 512:(half + 1) * 512],
                                           start=(dc == 0), stop=(dc == 7))
                        return ins
                    B.op("pe", mm, reads=[hnTcb[s2], wqb], writes=[C.bankb[qbk[half]]])

            def b_back(c):
                gc = tt * 4 + c
                s2 = gc % 2
                qbk = (1, 2) if c % 2 == 0 else (5, 6)
                for half in range(2):
                    qb_ = qbk[half]
                    B.op("act", lambda e, half=half, s2=s2, qb_=qb_: e.copy(out=Qb[s2][:, half * 512:(half + 1) * 512], in_=C.banks[qb_][:]),
                         reads=[C.bankb[qb_]], writes=[Qbb[s2]])
                    B.op("act", lambda e, half=half, qb_=qb_: e.activation(out=sq[:], in_=C.banks[qb_][:], func=AF.Square),
                         reads=[C.bankb[qb_]], writes=[sqb])
                    B.op("dve", lambda e, half=half, c=c: e.tensor_reduce(out=qn[:, c, 0, half * 4:(half + 1) * 4],
                                                                          in_=sq[:].rearrange("p (h d) -> p h d", h=4), axis=AX.X, op=ALU.add),
                         reads=[sqb], writes=[qnb[c]])
                emit_rope_sb(B, Qb[s2][:].rearrange("p (h d) -> p h d", h=8), Qbb[s2],
                             cos[:, gc, :], sin[:, gc, :], rtmp[s2], rtmpb[s2], ropeb)
                B.op("dve", lambda e, c=c: e.tensor_tensor(out=qn[:, c, 1, :], in0=qn[:, c, 0, :], in1=kmax2[:], op=ALU.mult),
                     reads=[qnb[c], kmaxb], writes=[qnb[c]])
                B.op("act", lambda e, c=c: e.activation(out=qn[:, c, 1, :], in_=qn[:, c, 1, :], func=AF.Sqrt),
                     reads=[qnb[c]], writes=[qnb[c]])
                B.op("dve", lambda e, c=c: e.tensor_scalar(out=qn[:, c, 2, :], in0=qn[:, c, 1, :], scalar1=-SCALE, scalar2=None, op0=ALU.mult),
                     reads=[qnb[c]], writes=[qnb[c]])
                emit_transpose8(B, C, Qb[s2], Qbb[s2], 0, ident, QT[:, :, c * 128:(c + 1) * 128], [QTb[c]], ident_buf=identb)

            b_front(0)
            for c in range(1, 4):
                b_front(c)
                b_back(c - 1)
            b_back(3)
            def att0(h, j):
                nonlocal gt_i
                qc = tt * 4 + j
                b = qc // 2
                g3 = gt_i % 3
                gt_i += 1
                if b >= 4:
                    qsl = QT[:, h, j * 128:(j + 1) * 128]
                    negb = qn[:, j, 2, h:h + 1]
                    B.op("pe", lambda e, qsl=qsl, h=h: e.matmul(C.banks[3][:, 0:NBLK], qsl, kmT[:, h, :], start=True, stop=True),
                         reads=[QTb[j], kmTb], writes=[C.bankb[3]])
                    g = gt[g3]
                    B.op("dve", lambda e, g=g, b=b: e.tensor_tensor(out=g[:, 0, :], in0=C.banks[3][:, 0:NBLK], in1=past[:, b, :], op=ALU.add),
                         reads=[C.bankb[3], pastb], writes=[gtb[g3]])
                    B.op("dve", lambda e, g=g: e.max(out=g[:, 1, 0:8], in_=g[:, 0, :]), reads=[gtb[g3]], writes=[gtb[g3]])
                    B.op("dve", lambda e, g=g: e.tensor_scalar(out=g[:, 2, :], in0=g[:, 0, :], scalar1=g[:, 1, 2:3], scalar2=None, op0=ALU.is_lt),
                         reads=[gtb[g3]], writes=[gtb[g3]])
                    B.op("dve", lambda e, g=g, negb=negb: e.scalar_tensor_tensor(
                        out=g[:, 3, :], in0=g[:, 2, :], scalar=NEG, in1=negb.broadcast_to([128, NBLK]), op0=ALU.mult, op1=ALU.add),
                        reads=[gtb[g3], qnb[j]], writes=[gtb[g3]])
                return dict(h=h, j=j, g3=g3)

            def att1(ctx):
                nonlocal hq_i, at_i
                h, j, g3 = ctx["h"], ctx["j"], ctx["g3"]
                if j == 0:
                    ks = hq_i % 2
                    hq_i += 1
                    B.dma("sp", KTh[ks][:, 0:nk_t * 128], T["KT"][h, :, 0:nk_t * 128], owner=KThb[ks], writes=[KThb[ks]])
                    B.dma("sp", Vh[ks][:, 0:nk_t, 0:HD], T["Vp"][h, :, 0:nk_t, :], owner=Vhb[ks], writes=[Vhb[ks]])
                ks = (hq_i - 1) % 2
                qc = tt * 4 + j
                b = qc // 2
                sub = qc % 2
                nkc = 2 * b + sub + 1
                a = at_i % 2
                at_i += 1
                qsl = QT[:, h, j * 128:(j + 1) * 128]
                negb = qn[:, j, 2, h:h + 1]
                use_sel = b >= 4
                npieces = (nkc + 3) // 4
                for kg in reversed(range(npieces)):
                    k0 = kg * 4
                    k1 = min(k0 + 4, nkc)
                    N = (k1 - k0) * 128
                    sbk = (5, 6, 1, 2)[self_sb[0] % 4]
                    self_sb[0] += 1
                    B.op("pe", lambda e, qsl=qsl, ks=ks, k0=k0, N=N, sbk=sbk: e.matmul(
                        C.banks[sbk][:, 0:N], qsl, KTh[ks][:, k0 * 128:k0 * 128 + N], start=True, stop=True),
                        reads=[QTb[j], KThb[ks]], writes=[C.bankb[sbk]])
                    chunks = []
                    kc = k0
                    while kc < k1:
                        w = 2 if (kc // 2) < b else 1
                        chunks.append((kc, w))
                        kc += w
                    for (kc, w) in reversed(chunks):
                        n_blk = kc // 2
                        if n_blk < b:
                            bias = gt[g3][:, 3, n_blk:n_blk + 1] if use_sel else negb
                            rd = [C.bankb[sbk], qnb[j]] + ([gtb[g3]] if use_sel else [])
                            B.op("act", lambda e, sbk=sbk, kc=kc, k0=k0, bias=bias, a=a: e.activation(
                                out=P[a][:, kc * 128:(kc + 2) * 128], in_=C.banks[sbk][:, (kc - k0) * 128:(kc - k0 + 2) * 128],
                                func=AF.Exp, scale=SCALE, bias=bias),
                                reads=rd, writes=[Pb[a]])
                        elif kc == nkc - 1:
                            B.op("dve", lambda e, sbk=sbk, kc=kc, k0=k0, a=a: e.tensor_tensor(
                                out=dg[a][:], in0=C.banks[sbk][:, (kc - k0) * 128:(kc - k0 + 1) * 128], in1=tri[:], op=ALU.add),
                                reads=[C.bankb[sbk], trib], writes=[dgb[a]])
                            B.op("act", lambda e, kc=kc, a=a, negb=negb: e.activation(
                                out=P[a][:, kc * 128:(kc + 1) * 128], in_=dg[a][:], func=AF.Exp, scale=SCALE, bias=negb),
                                reads=[dgb[a], qnb[j]], writes=[Pb[a]])
                        else:
                            B.op("act", lambda e, sbk=sbk, kc=kc, k0=k0, a=a, negb=negb: e.activation(
                                out=P[a][:, kc * 128:(kc + 1) * 128], in_=C.banks[sbk][:, (kc - k0) * 128:(kc - k0 + 1) * 128],
                                func=AF.Exp, scale=SCALE, bias=negb),
                                reads=[C.bankb[sbk], qnb[j]], writes=[Pb[a]])
                ctx.update(a=a, ks=ks, nkc=nkc)
                return ctx

            def att2(ctx):
                h, j, a, ks, nkc = (ctx[k] for k in ("h", "j", "a", "ks", "nkc"))
                ngr = (nkc + 7) // 8
                for gr in range(ngr):
                    c0 = gr * 8
                    c1 = min(c0 + 8, nkc)
                    tb = 7 if (self_tb[0] % 2 == 0) else 0
                    self_tb[0] += 1
                    pb_ = C.banks[tb][:].bitcast(BF16)

                    def trp(e, pb_=pb_, c0=c0, c1=c1, a=a):
                        ins = None
                        for kc in range(c0, c1):
                            ins = e.transpose(pb_[:, (kc - c0) * 128:(kc - c0 + 1) * 128], P[a][:, kc * 128:(kc + 1) * 128], ident[:])
                        return ins

                    B.op("pe", trp, reads=[Pb[a], identb], writes=[C.bankb[tb]])
                    B.op("dve", lambda e, pb_=pb_, c0=c0, c1=c1, a=a: e.tensor_copy(
                        out=PT[a][:, c0:c1, :], in_=pb_[:, 0:(c1 - c0) * 128].rearrange("p (c t) -> p c t", t=128)),
                        reads=[C.bankb[tb]], writes=[PTb[a][gr]])

                ctx["ngr"] = ngr
                return ctx

            def att3(ctx):
                h, j, a, ks, nkc, ngr = (ctx[k] for k in ("h", "j", "a", "ks", "nkc", "ngr"))

                def pv(e, a=a, ks=ks, nkc=nkc):
                    ins = None
                    for kc in range(nkc):
                        ins = e.matmul(C.banks[4][:, 0:HD + 1], PT[a][:, kc, :], Vh[ks][:, kc, :], start=(kc == 0), stop=(kc == nkc - 1))
                    return ins

                B.op("pe", pv, reads=[PTb[a][gr] for gr in range(ngr)] + [Vhb[ks], onesb], writes=[C.bankb[4]])
                B.op("dve", lambda e, a=a: e.reciprocal(out=rs[a][:, 0:1], in_=C.banks[4][:, HD:HD + 1]), reads=[C.bankb[4]], writes=[rsb[a]])
                B.op("dve", lambda e, a=a, j=j, h=h: e.tensor_scalar(out=Osb[j][:, h * 128:(h + 1) * 128], in0=C.banks[4][:, 0:HD],
                                                                     scalar1=rs[a][:, 0:1], scalar2=None, op0=ALU.mult),
                     reads=[C.bankb[4], rsb[a]], writes=[Osbb[j]])

            def p3a(c):
                o2 = c % 2
                emit_transpose8(B, C, Osb[c], Osbb[c], 0, ident, OT[o2][:, :, :], [OTb[o2]], ident_buf=identb)

            def p3b(c):
                o2 = c % 2
                obk = (1, 2) if c % 2 == 0 else (5, 6)
                for half in range(2):
                    ob_ = obk[half]

                    def mmo(e, half=half, o2=o2, ob_=ob_):
                        ins = None
                        for dc in range(8):
                            ins = e.matmul(C.banks[ob_][:], OT[o2][:, dc, :], wo[:, dc, half * 512:(half + 1) * 512],
                                           start=(dc == 0), stop=(dc == 7))
                        return ins
                    B.op("pe", mmo, reads=[OTb[o2], wob], writes=[C.bankb[ob_]])
                    ysl = y[:, c, half * 512:(half + 1) * 512]
                    B.op("dve", lambda e, ysl=ysl, ob_=ob_: e.tensor_tensor(out=ysl, in0=C.banks[ob_][:], in1=ysl, op=ALU.add),
                         reads=[C.bankb[ob_], ybufs[c]], writes=[ybufs[c]])
                B.dma("sp", T["h3"][t0 + c * 128:t0 + (c + 1) * 128, :], y[:, c, :], owner=ybufs[c], reads=[ybufs[c]])
                if tt < 7:
                    t1 = t0 + 512
                    B.dma("sp", y[:, c, :], T["h2"][t1 + c * 128:t1 + (c + 1) * 128, :], owner=ybufs[c], writes=[ybufs[c]])
                    B.op("act", lambda e, c=c: e.activation(out=junk[:], in_=y[:, c, :], func=AF.Square, accum_out=stat[:, 0, c:c + 1]),
                         reads=[ybufs[c]], writes=[statb])
                    if c == 3:
                        B.op("act", lambda e: e.activation(out=stat[:, 1, 0:4], in_=stat[:, 0, 0:4], func=AF.Sqrt, scale=1.0 / D, bias=EPS),
                             reads=[statb], writes=[statb])
                        B.op("dve", lambda e: e.reciprocal(out=stat[:, 2, 0:4], in_=stat[:, 1, 0:4]), reads=[statb], writes=[statb])
                if "o_dbg" in T:
                    B.dma("sp", T["o_dbg"][t0 + c * 128:t0 + (c + 1) * 128, :], Osb[c][:], owner=Osbb[c], reads=[Osbb[c]])

            items = [(h, j) for h in range(NH) for j in range(4)]
            n_it = len(items)
            c0s, c1s, c2s = {}, {}, {}
            for step in range(n_it + 3):
                if step < n_it:
                    c0s[step] = att0(*items[step])
                if 1 <= step <= n_it:
                    c1s[step - 1] = att1(c0s.pop(step - 1))
                if 2 <= step <= n_it + 1:
                    c2s[step - 2] = att2(c1s.pop(step - 2))
                if step >= 3:
                    att3(c2s.pop(step - 3))
                d = step - (n_it - 1)
                if 0 <= d <= 3:
                    p3a(d)
                    if d >= 1:
                        p3b(d - 1)
            p3b(3)
        B.end_stage()


def build(stages=("A1", "A2", "B", "C"), dbg=(), c_src="h3"):
    nc = bass.Bass("TRN2", target_bir_lowering=False)
    T = {}

    def inp(name, shape):
        T[name] = nc.dram_tensor(name, list(shape), F32, kind="ExternalInput").ap()

    def scratch(name, shape, dt, force_in=False):
        kind = "ExternalInput" if force_in else ("ExternalOutput" if name in dbg else "Internal")
        T[name] = nc.dram_tensor(name, list(shape), dt, kind=kind).ap()

    inp("attn_norm", (2, D))
    inp("ffn_norm", (2, D))
    inp("c_ident", (128, 128))
    if "A1" in stages:
        inp("x", (S, D))
        inp("pool_w", (1, 4, 256, 256))
        inp("pool_scale", (1, D))
        inp("ffn_w_gate", (1, D, F_DENSE))
        inp("ffn_w_up", (1, D, F_DENSE))
        inp("ffn_w_down", (1, F_DENSE, D))
        inp("c_band", (128, 12, 128))
    if "A2" in stages:
        inp("kv_norm", (D,))
        inp("wk", (D, D))
        inp("wv", (D, D))
    if "A2" in stages or "B" in stages:
        inp("c_cos", (128, NCH, 16))
        inp("c_sin", (128, NCH, 16))
        fin = ("A2" not in stages)
        kind = lambda n: "ExternalInput" if fin else ("ExternalOutput" if n in dbg else "Internal")
        T["KT"] = nc.dram_tensor("KT", [NH, HD, S], BF16, kind=kind("KT")).ap()
        T["Vp"] = nc.dram_tensor("Vp", [NH, 128, NCH, HD], BF16, kind=kind("Vp")).ap()
        T["kmT"] = nc.dram_tensor("kmT", [128, NH * NBLK], F32, kind=kind("kmT")).ap()
        T["kn2"] = nc.dram_tensor("kn2", [128, NH], F32, kind=kind("kn2")).ap()
    if "B" in stages:
        inp("wq", (1, D, D))
        inp("wo", (1, D, D))
        inp("c_tri", (128, 128))
        inp("c_past", (NBLK * NBLK,))
        if "o_dbg" in dbg:
            T["o_dbg"] = nc.dram_tensor("o_dbg", [S, D], BF16, kind="ExternalOutput").ap()
    if "C" in stages:
        inp("final_norm", (D,))
        inp("router_w", (1, D, NE))
        inp("moe_w_gate", (1, NE, D, F_MOE))
        inp("moe_w_up", (1, NE, D, F_MOE))
        inp("moe_w_down", (1, NE, F_MOE, D))
        T["out"] = nc.dram_tensor("out", [S, D], F32, kind="ExternalOutput").ap()
        if "gates" in dbg:
            scratch("gates", (S, NE), F32)
    scratch("h2", (S, D), F32, force_in=("A1" not in stages and ("A2" in stages or "B" in stages or c_src == "h2")))
    if "h1" in dbg:
        scratch("h1", (S, D), F32)
    if "B" in stages or "C" in stages:
        scratch("h3", (S, D), F32, force_in=("B" not in stages and "C" in stages and c_src == "h3"))
    B = Builder(nc)
    if "A1" in stages:
        stage_A1(nc, B, T)
    if "A2" in stages:
        stage_A2(nc, B, T)
    if "B" in stages:
        stage_B(nc, B, T)
    if "C" in stages:
        stage_C(nc, B, T, src=c_src)
    return nc, T


_WEIGHT_KEYS = ("attn_norm", "ffn_norm", "pool_w", "pool_scale", "wq", "wo", "kv_norm", "wk", "wv",
                "ffn_w_gate", "ffn_w_up", "ffn_w_down", "router_w", "moe_w_gate", "moe_w_up", "moe_w_down",
                "final_norm")


def kernel(**inputs):
    n_cores = 8
    nc, T = build(stages=("A1", "A2", "B", "C"))
    consts = make_consts()
    shared = {k: np.ascontiguousarray(np.asarray(inputs[k], dtype=np.float32)) for k in _WEIGHT_KEYS}
    shared.update(consts)
    x = np.asarray(inputs["x"], dtype=np.float32)
    in_maps = []
    for b in range(n_cores):
        m = dict(shared)
        m["x"] = np.ascontiguousarray(x[b])
        in_maps.append(m)
    res = run_bass_kernel_spmd(nc, in_maps, core_ids=list(range(n_cores)))
    out = np.stack([np.asarray(res.results[b]["out"], dtype=np.float32) for b in range(n_cores)], axis=0)
    return out
```

```python
import numpy as np
from contextlib import ExitStack
import concourse.bass as bass
import concourse.mybir as mybir
from concourse.bass_utils import run_bass_kernel_spmd

F32 = mybir.dt.float32
BF16 = mybir.dt.bfloat16
AF = mybir.ActivationFunctionType
ALU = mybir.AluOpType
AX = mybir.AxisListType

S = 4096
D = 1024
NCH = S // 128
NH = 8
HD = 128
BLK = 256
NBLK = S // BLK
F_DENSE = 2816
F_MOE = 3584
NE = 8
EPS = 1e-6
NEG = -30000.0
SCALE = HD ** -0.5


class Buf:
    __slots__ = ("name", "w", "r", "dsem", "dcnt", "excl")

    def __init__(self, name):
        self.name = name
        self.excl = False
        self.w = None
        self.r = {}
        self.dsem = {}
        self.dcnt = 0


class Builder:
    ENG = ("pe", "act", "dve", "pool", "sp")

    def __init__(self, nc):
        self.nc = nc
        self.esem = {e: nc.alloc_semaphore("es_" + e) for e in ("pe", "act", "dve", "pool")}
        self.ecnt = {e: 0 for e in self.esem}
        self.bar = nc.alloc_semaphore("bar")
        self.nbar = 0
        self.dpool = {"sp": [], "pool": []}
        self.nds = 0
        self._reset()

    def _reset(self):
        self.streams = {e: [] for e in self.ENG}
        self.waited = {e: {} for e in self.ENG}
        self.bufs = []

    def buf(self, name="b"):
        b = Buf(name)
        self.bufs.append(b)
        return b

    def bufs_n(self, n, name="b"):
        return [self.buf(f"{name}{i}") for i in range(n)]

    def _collect(self, eng, reads, writes):
        toks = {}

        def add(t):
            if t is None:
                return
            sem, val = t
            if toks.get(sem, (None, 0))[1] < val:
                toks[sem] = (sem, val)

        for b in reads:
            add(b.w)
            if b.excl:
                for t in b.r.values():
                    add(t)
        for b in writes:
            add(b.w)
            for t in b.r.values():
                add(t)
        out = []
        for sem, (_, val) in toks.items():
            if eng == "pe" and sem is self.esem["pe"]:
                continue
            if self.waited[eng].get(sem, 0) >= val:
                continue
            self.waited[eng][sem] = val
            out.append((sem, val))
        return out

    def _mark(self, tok, reads, writes):
        for b in writes:
            b.w = tok
            b.r = {}
        for b in reads:
            if b.r.get(tok[0], (None, 0))[1] < tok[1]:
                b.r[tok[0]] = tok

    def op(self, eng, fn, reads=(), writes=()):
        waits = self._collect(eng, reads, writes)
        self.ecnt[eng] += 1
        tok = (self.esem[eng], self.ecnt[eng])
        self.streams[eng].append((waits, fn, tok[0], 1))
        self._mark(tok, reads, writes)

    def dma(self, q, out, in_, owner, reads=(), writes=()):
        waits = self._collect(q, reads, writes)
        if q not in owner.dsem:
            if self.dpool[q]:
                owner.dsem[q] = list(self.dpool[q].pop())
            else:
                owner.dsem[q] = [self.nc.alloc_semaphore(f"ds{self.nds}"), 0]
                self.nds += 1
        owner.dsem[q][1] += 16
        tok = (owner.dsem[q][0], owner.dsem[q][1])
        self.streams[q].append((waits, (lambda e, o=out, i=in_: e.dma_start(out=o, in_=i)), tok[0], 16))
        self._mark(tok, reads, writes)

    def end_stage(self):
        waits = []
        for b in self.bufs:
            for (ds, dc) in b.dsem.values():
                waits.append((ds, dc))
        for e, s in self.esem.items():
            waits.append((s, self.ecnt[e]))
        self.nbar += 1
        nb = self.nbar
        bar = self.bar
        self.streams["sp"].append((waits, (lambda e: e.sem_inc(bar, 1)), None, 0))
        for e in self.ENG:
            self.streams[e].append(([(bar, nb)], None, None, 0))
        with self.nc.Block() as blk:
            for e, dec in (("pe", blk.tensor), ("act", blk.scalar), ("dve", blk.vector),
                           ("pool", blk.gpsimd), ("sp", blk.sync)):
                stream = self.streams[e]

                def body(eng, stream=stream):
                    for waits_, fn, sem, inc in stream:
                        for (s_, v_) in waits_:
                            eng.wait_ge(s_, v_)
                        if fn is not None:
                            ins = fn(eng)
                            if sem is not None:
                                ins.then_inc(sem, inc)

                dec(body)
        for b in self.bufs:
            for q, (ds, dc) in b.dsem.items():
                self.dpool[q].append((ds, dc))
        self._reset()


class Common:
    def __init__(self, B, es, nc, prefix):
        self.B = B
        self.nc = nc
        self.es = es
        self.prefix = prefix
        self.banks = [es.enter_context(nc.psum_tensor(f"{prefix}_bank{i}", [128, 512], F32)) for i in range(8)]
        self.bankb = B.bufs_n(8, "bank")
        for b in self.bankb:
            b.excl = True

    def sb(self, name, shape, dt):
        return self.es.enter_context(self.nc.sbuf_tensor(f"{self.prefix}_{name}", shape, dt))


def load_bcast(B, q, tile, vec_ap, buf):
    B.dma(q, tile[:], vec_ap.partition_broadcast(128), owner=buf, writes=[buf])


def emit_stats(B, C, srcs, src_bufs, stat, statbuf, junk):
    n = len(srcs)
    for i, (s, sbuf) in enumerate(zip(srcs, src_bufs)):
        B.op("act", lambda e, s=s, i=i: e.activation(out=junk[:], in_=s, func=AF.Square, accum_out=stat[:, 0, i:i + 1]),
             reads=[sbuf], writes=[statbuf])
    B.op("act", lambda e: e.activation(out=stat[:, 1, 0:n], in_=stat[:, 0, 0:n], func=AF.Sqrt, scale=1.0 / D, bias=EPS),
         reads=[statbuf], writes=[statbuf])
    B.op("dve", lambda e: e.reciprocal(out=stat[:, 2, 0:n], in_=stat[:, 1, 0:n]), reads=[statbuf], writes=[statbuf])


def emit_transpose8(B, C, src, src_buf, bank_i, ident, dst3, dst_bufs, evac_eng="act", ident_buf=None):
    pb = C.banks[bank_i][:].bitcast(BF16)
    bb = C.bankb[bank_i]

    def fn(e):
        ins = None
        for dc in range(8):
            ins = e.transpose(pb[:, dc * 128:(dc + 1) * 128], src[:, dc * 128:(dc + 1) * 128], ident[:])
        return ins

    B.op("pe", fn, reads=[src_buf] + ([ident_buf] if ident_buf is not None else []), writes=[bb])
    pv = pb.rearrange("p (c t) -> p c t", c=8)
    if evac_eng == "act":
        B.op("act", lambda e: e.copy(out=dst3, in_=pv), reads=[bb], writes=dst_bufs)
    else:
        B.op("dve", lambda e: e.tensor_copy(out=dst3, in_=pv), reads=[bb], writes=dst_bufs)


class FFN:
    def __init__(self, B, C, units, n_exp):
        self.B, self.C = B, C
        self.units = units
        self.n_exp = n_exp
        ufmax = max(n for _, n in units)
        self.ufmax = ufmax
        sb = C.sb
        self.NGU = 4
        self.wg = [sb(f"wg{i}", [128, 8, 256], BF16) for i in range(self.NGU)]
        self.wu = [sb(f"wu{i}", [128, 8, 256], BF16) for i in range(self.NGU)]
        self.wgb = B.bufs_n(self.NGU, "wg")
        self.wub = B.bufs_n(self.NGU, "wu")
        self.NWD = 2
        self.wd = [sb(f"wd{i}", [128, ufmax, 1024], BF16) for i in range(self.NWD)]
        self.wdb = B.bufs_n(self.NWD, "wd")
        self.actT = [sb(f"actT{i}", [128, ufmax, 1024], BF16) for i in range(2)]
        self.actb = [B.bufs_n(ufmax * 2, f"act{i}_") for i in range(2)]
        self.sg = [sb(f"sg{i}", [128, 512], BF16) for i in range(2)]
        self.sgb = B.bufs_n(2, "sg")
        self.gu_i = 0
        self.unit_i = 0
        self.pgu_i = 0
        self.pd_i = 0

    def run_tile(self, hnT, hnT_bufs, y, ybufs, wg_of, wu_of, wd_of, gates=None, gates_buf=None):
        B, C = self.B, self.C
        pending = None
        for e in range(self.n_exp):
            wg_d, wu_d, wd_d = wg_of(e), wu_of(e), wd_of(e)
            for (f0, n) in self.units:
                us = self.unit_i % 2
                ws = self.unit_i % self.NWD
                self.unit_i += 1
                for pr in range(n // 2):
                    gs = self.gu_i % self.NGU
                    self.gu_i += 1
                    c0 = (f0 + 2 * pr) * 128
                    B.dma("pool", self.wg[gs][:], wg_d[:, c0:c0 + 256].rearrange("(c p) f -> p c f", p=128),
                          owner=self.wgb[gs], writes=[self.wgb[gs]])
                    B.dma("pool", self.wu[gs][:], wu_d[:, c0:c0 + 256].rearrange("(c p) f -> p c f", p=128),
                          owner=self.wub[gs], writes=[self.wub[gs]])
                    for fl in range(2):
                        fci = 2 * pr + fl
                        for th in range(2):
                            k = self.pgu_i % 2
                            self.pgu_i += 1
                            pg, pu = C.banks[k], C.banks[2 + k]
                            pgb, pub = C.bankb[k], C.bankb[2 + k]

                            def mm(eng, ps=pg, w=self.wg[gs], fl=fl, th=th):
                                ins = None
                                for dc in range(8):
                                    ins = eng.matmul(ps[:], w[:, dc, fl * 128:(fl + 1) * 128],
                                                     hnT[:, dc, th * 512:(th + 1) * 512],
                                                     start=(dc == 0), stop=(dc == 7))
                                return ins

                            B.op("pe", mm, reads=[self.wgb[gs], hnT_bufs[th]], writes=[pgb])

                            def mm2(eng, ps=pu, w=self.wu[gs], fl=fl, th=th):
                                ins = None
                                for dc in range(8):
                                    ins = eng.matmul(ps[:], w[:, dc, fl * 128:(fl + 1) * 128],
                                                     hnT[:, dc, th * 512:(th + 1) * 512],
                                                     start=(dc == 0), stop=(dc == 7))
                                return ins

                            B.op("pe", mm2, reads=[self.wub[gs], hnT_bufs[th]], writes=[pub])
                            sgt, sgbuf = self.sg[k], self.sgb[k]
                            B.op("act", lambda eng, sgt=sgt, pg=pg: eng.activation(out=sgt[:], in_=pg[:], func=AF.Silu),
                                 reads=[pgb], writes=[sgbuf])
                            ab = self.actb[us][fci * 2 + th]
                            B.op("dve", lambda eng, sgt=sgt, pu=pu, a=self.actT[us], fci=fci, th=th:
                                 eng.tensor_tensor(out=a[:, fci, th * 512:(th + 1) * 512], in0=sgt[:], in1=pu[:], op=ALU.mult),
                                 reads=[sgbuf, pub], writes=[ab])
                B.dma("pool", self.wd[ws][:, 0:n, :],
                      wd_d[f0 * 128:(f0 + n) * 128, :].rearrange("(c p) d -> p c d", p=128),
                      owner=self.wdb[ws], writes=[self.wdb[ws]])
                if pending is not None:
                    self._down(*pending, y, ybufs, gates, gates_buf)
                pending = (us, ws, n, e)
        self._down(*pending, y, ybufs, gates, gates_buf)

    def _down(self, us, ws, n, e, y, ybufs, gates, gates_buf):
        B, C = self.B, self.C
        a, w = self.actT[us], self.wd[ws]
        for tc in range(8):
            th = tc // 4
            k = self.pd_i % 2
            self.pd_i += 1
            for dh in range(2):
                bi = 4 + 2 * k + dh
                ps, psb = C.banks[bi], C.bankb[bi]

                def mm(eng, ps=ps, tc=tc, dh=dh):
                    ins = None
                    for fci in range(n):
                        ins = eng.matmul(ps[:], a[:, fci, tc * 128:(tc + 1) * 128], w[:, fci, dh * 512:(dh + 1) * 512],
                                         start=(fci == 0), stop=(fci == n - 1))
                    return ins

                B.op("pe", mm, reads=[self.actb[us][fci * 2 + th] for fci in range(n)] + [self.wdb[ws]], writes=[psb])
                ysl = y[:, tc, dh * 512:(dh + 1) * 512]
                if gates is None:
                    B.op("dve", lambda eng, ysl=ysl, ps=ps: eng.tensor_tensor(out=ysl, in0=ps[:], in1=ysl, op=ALU.add),
                         reads=[psb, ybufs[tc]], writes=[ybufs[tc]])
                else:
                    B.op("dve", lambda eng, ysl=ysl, ps=ps, tc=tc, e=e: eng.scalar_tensor_tensor(
                        out=ysl, in0=ps[:], scalar=gates[:, tc, e:e + 1], in1=ysl, op0=ALU.mult, op1=ALU.add),
                        reads=[psb, ybufs[tc], gates_buf], writes=[ybufs[tc]])


def stage_A1(nc, B, T):
    with ExitStack() as es:
        C = Common(B, es, nc, "a1")
        sb = C.sb
        g1 = sb("g1", [128, D], F32)
        g2 = sb("g2", [128, D], F32)
        psc = sb("psc", [128, D], F32)
        band = sb("band", [128, 12, 128], BF16)
        ident = sb("ident", [128, 128], BF16)
        wp = sb("wp", [128, 8, 256], BF16)
        y = sb("y", [128, 8, D], F32)
        hn = [sb(f"hn{i}", [128, D], BF16) for i in range(3)]
        hn2 = [sb(f"hn2_{i}", [128, D], BF16) for i in range(2)]
        hnT = sb("hnT", [128, 8, 1024], BF16)
        pooledT = [sb(f"pooledT{i}", [128, 8, 128], BF16) for i in range(2)]
        junk = sb("junk", [128, D], BF16)
        stat = [sb(f"stat{i}", [128, 3, 8], F32) for i in range(2)]
        g1b, g2b, pscb, bandb, identb, wpb = (B.buf(n) for n in ("g1", "g2", "psc", "band", "ident", "wp"))
        ybufs = B.bufs_n(8, "y")
        hnb = B.bufs_n(3, "hn")
        hn2b = B.bufs_n(2, "hn2")
        hnTb = B.bufs_n(2, "hnT")
        pTb = B.bufs_n(2, "pooledT")
        statb = B.bufs_n(2, "stat")
        ffn = FFN(B, C, [(0, 6), (6, 6), (12, 6), (18, 4)], 1)

        load_bcast(B, "sp", g1, T["attn_norm"][0, :], g1b)
        load_bcast(B, "sp", g2, T["ffn_norm"][0, :], g2b)
        load_bcast(B, "sp", psc, T["pool_scale"][0, :], pscb)
        B.dma("pool", band[:], T["c_band"], owner=bandb, writes=[bandb])
        B.dma("pool", ident[:], T["c_ident"], owner=identb, writes=[identb])
        wp32 = y[:, 0:2, :].rearrange("p a (b d) -> p (a b) d", d=256)
        B.dma("sp", wp32, T["pool_w"][0].rearrange("g (k p) d -> p (g k) d", p=128), owner=ybufs[0],
              writes=[ybufs[0], ybufs[1]])
        for g in range(4):
            B.op("dve", lambda e, g=g: e.tensor_tensor(
                out=wp[:, 2 * g:2 * g + 2, :], in0=wp32[:, 2 * g:2 * g + 2, :],
                in1=psc[:, g * 256:(g + 1) * 256].rearrange("p (o d) -> p o d", o=1).broadcast_to([128, 2, 256]),
                op=ALU.mult), reads=[ybufs[0], ybufs[1], pscb], writes=[wpb])

        gchunk = 0
        for tt in range(4):
            t0 = tt * 1024
            for c in range(8):
                B.dma("sp", y[:, c, :], T["x"][t0 + c * 128:t0 + (c + 1) * 128, :], owner=ybufs[c], writes=[ybufs[c]])
            st, stb = stat[0], statb[0]
            emit_stats(B, C, [y[:, c, :] for c in range(8)], ybufs, st, stb, junk)
            def a1_front(c, g, st=st):
                hs = g % 3
                hp = (g - 1) % 3
                pb0 = 4 if g % 2 == 0 else 0
                B.op("dve", lambda e, c=c, hs=hs, st=st: e.scalar_tensor_tensor(
                    out=hn[hs][:], in0=y[:, c, :], scalar=st[:, 2, c:c + 1], in1=g1[:], op0=ALU.mult, op1=ALU.mult),
                    reads=[ybufs[c], stb, g1b], writes=[hnb[hs]])
                first = (g == 0)

                def pool_mm(e, hs=hs, hp=hp, first=first, pb0=pb0):
                    ins = None
                    for dc in range(8):
                        w = dc // 2
                        out = C.banks[pb0 + dc // 4][:, (dc % 4) * 128:(dc % 4 + 1) * 128]
                        if first:
                            ins = e.matmul(out, hn[hs][:, dc * 128:(dc + 1) * 128], band[:, 8 + w, :], start=True, stop=True)
                        else:
                            e.matmul(out, hn[hp][:, dc * 128:(dc + 1) * 128], band[:, 4 + w, :], start=True, stop=False)
                            ins = e.matmul(out, hn[hs][:, dc * 128:(dc + 1) * 128], band[:, w, :], start=False, stop=True)
                    return ins

                B.op("pe", pool_mm, reads=[hnb[hs], bandb] + ([] if first else [hnb[hp]]), writes=[C.bankb[pb0], C.bankb[pb0 + 1]])

            def a1_back(c, g):
                pb0 = 4 if g % 2 == 0 else 0
                ps_ = g % 2
                for hb in range(2):
                    B.op("act", lambda e, ps_=ps_, hb=hb, pb0=pb0: e.copy(
                        out=pooledT[ps_][:, hb * 4:(hb + 1) * 4, :],
                        in_=C.banks[pb0 + hb][:].rearrange("p (c t) -> p c t", c=4)),
                        reads=[C.bankb[pb0 + hb]], writes=[pTb[ps_]])

                def y_mm(e, ps_=ps_):
                    ins = None
                    for g_ in range(4):
                        out = C.banks[6 + g_ // 2][:, (g_ % 2) * 256:(g_ % 2 + 1) * 256]
                        for k in range(2):
                            ins = e.matmul(out, pooledT[ps_][:, 2 * g_ + k, :], wp[:, 2 * g_ + k, :], start=(k == 0), stop=(k == 1))
                    return ins

                B.op("pe", y_mm, reads=[pTb[ps_], wpb], writes=[C.bankb[6], C.bankb[7]])
                for hb in range(2):
                    ysl = y[:, c, hb * 512:(hb + 1) * 512]
                    B.op("dve", lambda e, ysl=ysl, hb=hb: e.tensor_tensor(out=ysl, in0=C.banks[6 + hb][:], in1=ysl, op=ALU.add),
                         reads=[C.bankb[6 + hb], ybufs[c]], writes=[ybufs[c]])

            a1_front(0, gchunk)
            for c in range(1, 8):
                a1_front(c, gchunk + c)
                a1_back(c - 1, gchunk + c - 1)
            a1_back(7, gchunk + 7)
            gchunk += 8
            if "h1" in T:
                for c in range(8):
                    B.dma("sp", T["h1"][t0 + c * 128:t0 + (c + 1) * 128, :], y[:, c, :], owner=ybufs[c], reads=[ybufs[c]])
            st, stb = stat[1], statb[1]
            emit_stats(B, C, [y[:, c, :] for c in range(8)], ybufs, st, stb, junk)
            for c in range(8):
                hs = c % 2
                B.op("dve", lambda e, c=c, hs=hs, st=st: e.scalar_tensor_tensor(
                    out=hn2[hs][:], in0=y[:, c, :], scalar=st[:, 2, c:c + 1], in1=g2[:], op0=ALU.mult, op1=ALU.mult),
                    reads=[ybufs[c], stb, g2b], writes=[hn2b[hs]])
                emit_transpose8(B, C, hn2[hs], hn2b[hs], 0 + (c % 2), ident,
                                hnT[:, :, c * 128:(c + 1) * 128], [hnTb[c // 4]], ident_buf=identb)
            ffn.run_tile(hnT, hnTb, y, ybufs,
                         lambda e: T["ffn_w_gate"][0], lambda e: T["ffn_w_up"][0], lambda e: T["ffn_w_down"][0])
            for c in range(8):
                B.dma("sp", T["h2"][t0 + c * 128:t0 + (c + 1) * 128, :], y[:, c, :], owner=ybufs[c], reads=[ybufs[c]])
        B.end_stage()


def make_consts():
    band = np.zeros((128, 12, 128), np.float32)
    s = np.arange(128)[:, None]
    t = np.arange(128)[None, :]
    for wi, w in enumerate((2, 4, 8, 16)):
        dlt = t - s
        band[:, wi, :] = np.where((dlt >= 0) & (dlt < w), 1.0 / w, 0.0) - (dlt == 0)
        band[:, 4 + wi, :] = np.where((t + 128 - s) < w, 1.0 / w, 0.0)
        cnt = np.minimum(t + 1, w).astype(np.float32)
        band[:, 8 + wi, :] = np.where((dlt >= 0) & (dlt < w), 1.0 / cnt, 0.0) - (dlt == 0)
    ident = np.eye(128, dtype=np.float32)
    half = 16
    inv = (np.float32(500000.0) ** (-np.arange(half, dtype=np.float32) * np.float32(2.0) / np.float32(32))).astype(np.float32)
    ang = (np.arange(S, dtype=np.float32)[:, None] * inv[None, :]).astype(np.float32)
    cos8, sin8 = np.cos(ang.astype(np.float64)).astype(np.float32), np.sin(ang.astype(np.float64)).astype(np.float32)
    q = np.arange(128)[:, None]
    k = np.arange(128)[None, :]
    tri = np.where(k <= q, 0.0, NEG).astype(np.float32)
    bb = np.arange(NBLK)[:, None]
    nn = np.arange(NBLK)[None, :]
    past = np.where(nn < bb, 0.0, -1e30).astype(np.float32).reshape(-1)
    cos8 = np.ascontiguousarray(cos8.reshape(NCH, 128, 16).transpose(1, 0, 2))
    sin8 = np.ascontiguousarray(sin8.reshape(NCH, 128, 16).transpose(1, 0, 2))
    return {"c_band": band, "c_ident": ident, "c_cos": cos8, "c_sin": sin8, "c_tri": tri, "c_past": past}


def stage_C(nc, B, T, src="h3"):
    with ExitStack() as es:
        C = Common(B, es, nc, "c")
        sb = C.sb
        g2 = sb("g2", [128, D], F32)
        gf = sb("gf", [128, D], F32)
        ident32 = sb("ident32", [128, 128], F32)
        wr = sb("wr", [128, 8, NE], F32)
        y = sb("y", [128, 8, D], F32)
        hnf = [sb(f"hnf{i}", [128, D], F32) for i in range(2)]
        hnT = sb("hnT", [128, 8, 1024], BF16)
        hnT32s = [sb(f"hnT32_{i}", [128, 8, 128], F32) for i in range(2)]
        gates = sb("gates", [128, 8, NE], F32)
        rt = [sb(f"rt{i}", [128, 6, NE], F32) for i in range(2)]
        junk = sb("junk", [128, D], BF16)
        stat = [sb(f"stat{i}", [128, 3, 8], F32) for i in range(2)]
        g2b, gfb, id32b, wrb, gatesb = (B.buf(n) for n in ("g2", "gf", "id32", "wr", "gates"))
        hnT32bs = B.bufs_n(2, "hnT32")
        ybufs = B.bufs_n(8, "y")
        hnfb = B.bufs_n(2, "hnf")
        hnTb = B.bufs_n(2, "hnT")
        rtb = B.bufs_n(2, "rt")
        statb = B.bufs_n(2, "stat")
        ffn = FFN(B, C, [(0, 8), (8, 8), (16, 8), (24, 4)], NE)

        load_bcast(B, "sp", g2, T["ffn_norm"][1, :], g2b)
        load_bcast(B, "sp", gf, T["final_norm"], gfb)
        B.dma("sp", ident32[:], T["c_ident"], owner=id32b, writes=[id32b])
        B.dma("sp", wr[:], T["router_w"][0].rearrange("(c p) e -> p c e", p=128), owner=wrb, writes=[wrb])

        for tt in range(4):
            t0 = tt * 1024
            if tt == 0:
                for c in range(8):
                    B.dma("sp", y[:, c, :], T[src][t0 + c * 128:t0 + (c + 1) * 128, :], owner=ybufs[c], writes=[ybufs[c]])
            st, stb = stat[0], statb[0]
            emit_stats(B, C, [y[:, c, :] for c in range(8)], ybufs, st, stb, junk)
            def c_front(c, st=st):
                hs = c % 2
                hnT32, hnT32b = hnT32s[c % 2], hnT32bs[c % 2]
                B.op("dve", lambda e, c=c, hs=hs, st=st: e.scalar_tensor_tensor(
                    out=hnf[hs][:], in0=y[:, c, :], scalar=st[:, 2, c:c + 1], in1=g2[:], op0=ALU.mult, op1=ALU.mult),
                    reads=[ybufs[c], stb, g2b], writes=[hnfb[hs]])

                def tr(e, hs=hs):
                    ins = None
                    for dc in range(8):
                        ins = e.transpose(C.banks[4 + dc // 4][:, (dc % 4) * 128:(dc % 4 + 1) * 128],
                                          hnf[hs][:, dc * 128:(dc + 1) * 128], ident32[:])
                    return ins

                B.op("pe", tr, reads=[hnfb[hs], id32b], writes=[C.bankb[4], C.bankb[5]])
                for hb in range(2):
                    pv = C.banks[4 + hb][:].rearrange("p (c t) -> p c t", c=4)
                    B.op("act", lambda e, pv=pv, hb=hb, c=c: e.copy(out=hnT[:, hb * 4:(hb + 1) * 4, c * 128:(c + 1) * 128], in_=pv),
                         reads=[C.bankb[4 + hb]], writes=[hnTb[c // 4]])
                    B.op("dve", lambda e, pv=pv, hb=hb: e.tensor_copy(out=hnT32[:, hb * 4:(hb + 1) * 4, :], in_=pv),
                         reads=[C.bankb[4 + hb]], writes=[hnT32b])

            def c_back(c):
                hnT32, hnT32b = hnT32s[c % 2], hnT32bs[c % 2]
                def lg_mm(e):
                    ins = None
                    for dc in range(8):
                        ins = e.matmul(C.banks[6][:, 0:NE], hnT32[:, dc, :], wr[:, dc, :], start=(dc == 0), stop=(dc == 7))
                    return ins

                B.op("pe", lg_mm, reads=[hnT32b, wrb], writes=[C.bankb[6]])
                r, rb = rt[c % 2], rtb[c % 2]
                B.op("dve", lambda e, r=r: e.tensor_copy(out=r[:, 0, :], in_=C.banks[6][:, 0:NE]), reads=[C.bankb[6]], writes=[rb])
                B.op("dve", lambda e, r=r: e.max(out=r[:, 1, :], in_=r[:, 0, :]), reads=[rb], writes=[rb])
                B.op("dve", lambda e, r=r: e.tensor_scalar(out=r[:, 2, :], in0=r[:, 0, :], scalar1=r[:, 1, 0:1], scalar2=None,
                                                           op0=ALU.subtract), reads=[rb], writes=[rb])
                B.op("dve", lambda e, r=r: e.tensor_scalar(out=r[:, 3, :], in0=r[:, 0, :], scalar1=r[:, 1, 1:2], scalar2=None,
                                                           op0=ALU.is_ge), reads=[rb], writes=[rb])
                B.op("act", lambda e, r=r: e.activation(out=r[:, 2, :], in_=r[:, 2, :], func=AF.Exp), reads=[rb], writes=[rb])
                B.op("dve", lambda e, r=r: e.tensor_tensor(out=r[:, 4, :], in0=r[:, 2, :], in1=r[:, 3, :], op=ALU.mult),
                     reads=[rb], writes=[rb])
                B.op("dve", lambda e, r=r: e.tensor_reduce(out=r[:, 5, 0:1], in_=r[:, 4, :], axis=AX.X, op=ALU.add),
                     reads=[rb], writes=[rb])
                B.op("dve", lambda e, r=r: e.reciprocal(out=r[:, 5, 1:2], in_=r[:, 5, 0:1]), reads=[rb], writes=[rb])
                B.op("dve", lambda e, r=r, c=c: e.tensor_scalar(out=gates[:, c, :], in0=r[:, 4, :], scalar1=r[:, 5, 1:2], scalar2=None,
                                                                op0=ALU.mult), reads=[rb], writes=[gatesb])

            c_front(0)
            for c in range(1, 8):
                c_front(c)
                c_back(c - 1)
            c_back(7)
            if "gates" in T:
                B.dma("sp", T["gates"][t0:t0 + 1024, :].rearrange("(c p) e -> p c e", p=128), gates[:], owner=gatesb, reads=[gatesb])
            ffn.run_tile(hnT, hnTb, y, ybufs,
                         lambda e: T["moe_w_gate"][0, e], lambda e: T["moe_w_up"][0, e], lambda e: T["moe_w_down"][0, e],
                         gates=gates, gates_buf=gatesb)
            st, stb = stat[1], statb[1]
            emit_stats(B, C, [y[:, c, :] for c in range(8)], ybufs, st, stb, junk)
            for c in range(8):
                hs = c % 2
                B.op("dve", lambda e, c=c, hs=hs, st=st: e.scalar_tensor_tensor(
                    out=hnf[hs][:], in0=y[:, c, :], scalar=st[:, 2, c:c + 1], in1=gf[:], op0=ALU.mult, op1=ALU.mult),
                    reads=[ybufs[c], stb, gfb], writes=[hnfb[hs]])
                B.dma("sp", T["out"][t0 + c * 128:t0 + (c + 1) * 128, :], hnf[hs][:], owner=hnfb[hs], reads=[hnfb[hs]])
                if tt < 3:
                    t1 = t0 + 1024
                    B.dma("sp", y[:, c, :], T[src][t1 + c * 128:t1 + (c + 1) * 128, :], owner=ybufs[c], writes=[ybufs[c]])
        B.end_stage()


def emit_rope(B, src_bank, src_bankb, dst3, dst_buf, cos_c, sin_c, tmp, tmpb, ropeb):
    pv = src_bank[:].rearrange("p (h d) -> p h d", h=4)
    x1, x2 = pv[:, :, 0:16], pv[:, :, 16:32]
    cb = cos_c.rearrange("p (o k) -> p o k", o=1).broadcast_to([128, 4, 16])
    sbb = sin_c.rearrange("p (o k) -> p o k", o=1).broadcast_to([128, 4, 16])
    for i, (a, b_) in enumerate(((x1, cb), (x2, sbb), (x2, cb), (x1, sbb))):
        B.op("dve", lambda e, a=a, b_=b_, i=i: e.tensor_tensor(out=tmp[:, i, :, :], in0=a, in1=b_, op=ALU.mult),
             reads=[src_bankb, ropeb], writes=[tmpb])
    B.op("dve", lambda e: e.tensor_tensor(out=dst3[:, :, 0:16], in0=tmp[:, 0, :, :], in1=tmp[:, 1, :, :], op=ALU.subtract),
         reads=[tmpb], writes=[dst_buf])
    B.op("dve", lambda e: e.tensor_tensor(out=dst3[:, :, 16:32], in0=tmp[:, 2, :, :], in1=tmp[:, 3, :, :], op=ALU.add),
         reads=[tmpb], writes=[dst_buf])


def emit_rope_sb(B, dst3, dst_buf, cos_c, sin_c, tmp, tmpb, ropeb):
    x1, x2 = dst3[:, :, 0:16], dst3[:, :, 16:32]
    cb = cos_c.rearrange("p (o k) -> p o k", o=1).broadcast_to([128, NH, 16])
    sbb = sin_c.rearrange("p (o k) -> p o k", o=1).broadcast_to([128, NH, 16])
    for i, (a, b_) in enumerate(((x1, cb), (x2, sbb), (x2, cb), (x1, sbb))):
        B.op("dve", lambda e, a=a, b_=b_, i=i: e.tensor_tensor(out=tmp[:, i, :, :], in0=a, in1=b_, op=ALU.mult),
             reads=[dst_buf, ropeb], writes=[tmpb])
    B.op("dve", lambda e: e.tensor_tensor(out=x1, in0=tmp[:, 0, :, :], in1=tmp[:, 1, :, :], op=ALU.subtract),
         reads=[tmpb], writes=[dst_buf])
    B.op("dve", lambda e: e.tensor_tensor(out=x2, in0=tmp[:, 2, :, :], in1=tmp[:, 3, :, :], op=ALU.add),
         reads=[tmpb], writes=[dst_buf])


def stage_A2(nc, B, T):
    with ExitStack() as es:
        C = Common(B, es, nc, "a2")
        sb = C.sb
        gk = sb("gk", [128, D], F32)
        ident = sb("ident", [128, 128], BF16)
        wk = sb("wk", [128, 8, D], BF16)
        wv = sb("wv", [128, 8, D], BF16)
        cos = sb("cos", [128, NCH, 16], F32)
        sin = sb("sin", [128, NCH, 16], F32)
        ones = sb("ones", [128, 1], BF16)
        y = sb("y", [128, 8, D], F32)
        hn = [sb(f"hn{i}", [128, D], BF16) for i in range(2)]
        hnTc = [sb(f"hnTc{i}", [128, 8, 128], BF16) for i in range(2)]
        Kb = [sb(f"Kb{i}", [128, D], BF16) for i in range(2)]
        Vb = [sb(f"Vb{i}", [128, D], BF16) for i in range(2)]
        KT4 = [sb(f"KT4_{i}", [128, 8, 512], BF16) for i in range(2)]
        rtmp = [sb(f"rtmp{i}", [128, 4, NH, 16], F32) for i in range(2)]
        sq = sb("sq", [128, 512], F32)
        kn2 = sb("kn2", [128, 2, 8], F32)
        kmT = sb("kmT", [128, 8, NBLK], F32)
        junk = sb("junk", [128, D], BF16)
        stat = sb("stat", [128, 3, 8], F32)
        gkb, identb, wkb, wvb, ropeb, onesb, kn2b, kmTb, statb, sqb = (
            B.buf(n) for n in ("gk", "ident", "wk", "wv", "rope", "ones", "kn2", "kmT", "stat", "sq"))
        ybufs = B.bufs_n(8, "y")
        hnb = B.bufs_n(2, "hn")
        hnTcb = B.bufs_n(2, "hnTc")
        Kbb = B.bufs_n(2, "Kb")
        Vbb = B.bufs_n(2, "Vb")
        KT4b = B.bufs_n(2, "KT4")
        rtmpb = B.bufs_n(2, "rtmp")

        load_bcast(B, "sp", gk, T["kv_norm"], gkb)
        B.dma("pool", ident[:], T["c_ident"], owner=identb, writes=[identb])
        B.dma("pool", wk[:], T["wk"].rearrange("(c p) n -> p c n", p=128), owner=wkb, writes=[wkb])
        B.dma("pool", wv[:], T["wv"].rearrange("(c p) n -> p c n", p=128), owner=wvb, writes=[wvb])
        B.dma("sp", cos[:], T["c_cos"], owner=ropeb, writes=[ropeb])
        B.dma("sp", sin[:], T["c_sin"], owner=ropeb, writes=[ropeb])
        B.op("dve", lambda e: e.memset(ones[:], 1.0 / BLK), writes=[onesb])
        B.op("dve", lambda e: e.memset(kn2[:], 0.0), writes=[kn2b])

        for tt in range(4):
            t0 = tt * 1024
            for c in range(8):
                B.dma("sp", y[:, c, :], T["h2"][t0 + c * 128:t0 + (c + 1) * 128, :], owner=ybufs[c], writes=[ybufs[c]])
            emit_stats(B, C, [y[:, c, :] for c in range(8)], ybufs, stat, statb, junk)
            def a2_front(c):
                gc = tt * 8 + c
                s2 = gc % 2
                kbk = (1, 2) if gc % 2 == 0 else (7, 6)
                B.op("dve", lambda e, c=c, s2=s2: e.scalar_tensor_tensor(
                    out=hn[s2][:], in0=y[:, c, :], scalar=stat[:, 2, c:c + 1], in1=gk[:], op0=ALU.mult, op1=ALU.mult),
                    reads=[ybufs[c], statb, gkb], writes=[hnb[s2]])
                emit_transpose8(B, C, hn[s2], hnb[s2], 0, ident, hnTc[s2][:, :, :], [hnTcb[s2]], ident_buf=identb)
                for (w, wb, bks) in ((wk, wkb, kbk),):
                    for half in range(2):
                        def mm(e, w=w, half=half, bk=bks[half], s2=s2):
                            ins = None
                            for dc in range(8):
                                ins = e.matmul(C.banks[bk][:], hnTc[s2][:, dc, :], w[:, dc, half * 512:(half + 1) * 512],
                                               start=(dc == 0), stop=(dc == 7))
                            return ins
                        B.op("pe", mm, reads=[hnTcb[s2], wb], writes=[C.bankb[bks[half]]])

            def a2_back(c):
                gc = tt * 8 + c
                s2 = gc % 2
                kbk = (1, 2) if gc % 2 == 0 else (7, 6)
                for half in range(2):
                    def mmv(e, half=half, s2=s2):
                        ins = None
                        for dc in range(8):
                            ins = e.matmul(C.banks[3 + half][:], hnTc[s2][:, dc, :], wv[:, dc, half * 512:(half + 1) * 512],
                                           start=(dc == 0), stop=(dc == 7))
                        return ins
                    B.op("pe", mmv, reads=[hnTcb[s2], wvb], writes=[C.bankb[3 + half]])
                for half in range(2):
                    B.op("act", lambda e, half=half, s2=s2: e.copy(out=Vb[s2][:, half * 512:(half + 1) * 512], in_=C.banks[3 + half][:]),
                         reads=[C.bankb[3 + half]], writes=[Vbb[s2]])
                B.dma("sp", T["Vp"][:, :, gc, :].rearrange("h p d -> p h d"), Vb[s2][:].rearrange("p (h d) -> p h d", h=8),
                      owner=Vbb[s2], reads=[Vbb[s2]])
                for half in range(2):
                    kb_ = kbk[half]
                    B.op("act", lambda e, half=half, s2=s2, kb_=kb_: e.copy(out=Kb[s2][:, half * 512:(half + 1) * 512], in_=C.banks[kb_][:]),
                         reads=[C.bankb[kb_]], writes=[Kbb[s2]])
                    B.op("act", lambda e, half=half, kb_=kb_: e.activation(out=sq[:], in_=C.banks[kb_][:], func=AF.Square),
                         reads=[C.bankb[kb_]], writes=[sqb])
                    B.op("dve", lambda e, half=half: e.tensor_reduce(out=kn2[:, 0, half * 4:(half + 1) * 4],
                                                                     in_=sq[:].rearrange("p (h d) -> p h d", h=4), axis=AX.X, op=ALU.add),
                         reads=[sqb], writes=[kn2b])
                emit_rope_sb(B, Kb[s2][:].rearrange("p (h d) -> p h d", h=8), Kbb[s2],
                             cos[:, gc, :], sin[:, gc, :], rtmp[s2], rtmpb[s2], ropeb)
                B.op("dve", lambda e: e.tensor_tensor(out=kn2[:, 1, :], in0=kn2[:, 1, :], in1=kn2[:, 0, :], op=ALU.max),
                     reads=[kn2b], writes=[kn2b])
                k4 = (gc // 4) % 2
                j4 = gc % 4
                emit_transpose8(B, C, Kb[s2], Kbb[s2], 5, ident, KT4[k4][:, :, j4 * 128:(j4 + 1) * 128], [KT4b[k4]], evac_eng="act", ident_buf=identb)
                if j4 == 3:
                    tq = (gc // 4) * 512
                    B.dma("sp", T["KT"][:, :, tq:tq + 512].rearrange("h p t -> p h t"), KT4[k4][:], owner=KT4b[k4], reads=[KT4b[k4]])
                if gc % 2 == 1:
                    blk = gc // 2
                    sp_ = (gc - 1) % 2

                    def km(e, sp_=sp_, s2=s2):
                        ins = None
                        for h in range(8):
                            e.matmul(C.banks[0][:, h:h + 1], Kb[sp_][:, h * 128:(h + 1) * 128], ones[:], start=True, stop=False)
                            ins = e.matmul(C.banks[0][:, h:h + 1], Kb[s2][:, h * 128:(h + 1) * 128], ones[:], start=False, stop=True)
                        return ins

                    B.op("pe", km, reads=[Kbb[sp_], Kbb[s2], onesb], writes=[C.bankb[0]])
                    B.op("dve", lambda e, blk=blk: e.tensor_copy(out=kmT[:, :, blk], in_=C.banks[0][:, 0:8]),
                         reads=[C.bankb[0]], writes=[kmTb])

            a2_front(0)
            for c in range(1, 8):
                a2_front(c)
                a2_back(c - 1)
            a2_back(7)
        B.dma("sp", T["kmT"], kmT[:].rearrange("p h n -> p (h n)"), owner=kmTb, reads=[kmTb])
        B.dma("sp", T["kn2"], kn2[:, 1, :], owner=kn2b, reads=[kn2b])
        B.end_stage()


def stage_B(nc, B, T):
    with ExitStack() as es:
        C = Common(B, es, nc, "b")
        sb = C.sb
        g1 = sb("g1", [128, D], F32)
        ident = sb("ident", [128, 128], BF16)
        wq = sb("wq", [128, 8, D], BF16)
        wo = sb("wo", [128, 8, D], BF16)
        cos = sb("cos", [128, NCH, 16], F32)
        sin = sb("sin", [128, NCH, 16], F32)
        tri = sb("tri", [128, 128], F32)
        past = sb("past", [128, NBLK, NBLK], F32)
        kmT32 = sb("kmT32", [128, NH * NBLK], F32)
        kmT = sb("kmT", [128, NH, NBLK], BF16)
        kn2all = sb("kn2all", [128, 128 * NH], F32)
        kmax2 = sb("kmax2", [128, NH], F32)
        y = sb("y", [128, 4, D], F32)
        hn = [sb(f"hn{i}", [128, D], BF16) for i in range(2)]
        hnTc = [sb(f"hnTc{i}", [128, 8, 128], BF16) for i in range(2)]
        Qb = [sb(f"Qb{i}", [128, D], BF16) for i in range(2)]
        QT = sb("QT", [128, NH, 512], BF16)
        rtmp = [sb(f"rtmp{i}", [128, 4, NH, 16], F32) for i in range(2)]
        sq = sb("sq", [128, 512], F32)
        qn = sb("qn", [128, 4, 3, NH], F32)
        KTh = [sb(f"KTh{i}", [128, S], BF16) for i in range(2)]
        Vh = [sb(f"Vh{i}", [128, NCH, HD + 1], BF16) for i in range(2)]
        P = [sb(f"P{i}", [128, S], BF16) for i in range(2)]
        PT = [sb(f"PT{i}", [128, NCH, 128], BF16) for i in range(2)]
        dg = [sb(f"dg{i}", [128, 128], F32) for i in range(2)]
        gt = [sb(f"gt{i}", [128, 4, NBLK], F32) for i in range(3)]
        rs = [sb(f"rs{i}", [128, 24], F32) for i in range(2)]
        Osb = [sb(f"Osb{i}", [128, D], BF16) for i in range(4)]
        OT = [sb(f"OT{i}", [128, 8, 128], BF16) for i in range(2)]
        junk = sb("junk", [128, D], BF16)
        stat = sb("stat", [128, 3, 8], F32)
        (g1b, identb, wqb, wob, ropeb, trib, pastb, kmTb, kmaxb, QTb_unused, statb, sqb) = (
            B.buf(n) for n in ("g1", "ident", "wq", "wo", "rope", "tri", "past", "kmT", "kmax", "x", "stat", "sq"))
        ybufs = B.bufs_n(4, "y")
        hnb = B.bufs_n(2, "hn")
        hnTcb = B.bufs_n(2, "hnTc")
        Qbb = B.bufs_n(2, "Qb")
        QTb = B.bufs_n(4, "QT")
        rtmpb = B.bufs_n(2, "rtmp")
        qnb = B.bufs_n(4, "qn")
        KThb = B.bufs_n(2, "KTh")
        Vhb = B.bufs_n(2, "Vh")
        Pb = B.bufs_n(2, "P")
        PTb = [B.bufs_n(4, f"PT{i}_") for i in range(2)]
        dgb = B.bufs_n(2, "dg")
        gtb = B.bufs_n(3, "gt")
        rsb = B.bufs_n(2, "rs")
        onesb = B.buf("vones")
        Osbb = B.bufs_n(4, "Osb")
        OTb = B.bufs_n(2, "OT")

        load_bcast(B, "sp", g1, T["attn_norm"][1, :], g1b)
        B.dma("pool", ident[:], T["c_ident"], owner=identb, writes=[identb])
        B.dma("pool", wq[:], T["wq"][0].rearrange("(c p) n -> p c n", p=128), owner=wqb, writes=[wqb])
        B.dma("pool", wo[:], T["wo"][0].rearrange("(c p) n -> p c n", p=128), owner=wob, writes=[wob])
        B.dma("sp", cos[:], T["c_cos"], owner=ropeb, writes=[ropeb])
        B.dma("sp", sin[:], T["c_sin"], owner=ropeb, writes=[ropeb])
        B.dma("sp", tri[:], T["c_tri"], owner=trib, writes=[trib])
        B.dma("sp", past[:].rearrange("p a b -> p (a b)"), T["c_past"].partition_broadcast(128), owner=pastb, writes=[pastb])
        B.dma("sp", kmT32[:], T["kmT"], owner=kmTb, writes=[kmTb])
        B.op("dve", lambda e: e.tensor_copy(out=kmT[:].rearrange("p h n -> p (h n)"), in_=kmT32[:]), reads=[kmTb], writes=[kmTb])
        B.dma("sp", kn2all[:], T["kn2"].rearrange("p h -> (p h)").partition_broadcast(128), owner=kmaxb, writes=[kmaxb])
        B.op("dve", lambda e: e.tensor_reduce(out=kmax2[:], in_=kn2all[:].rearrange("q (p h) -> q h p", h=NH), axis=AX.X, op=ALU.max),
             reads=[kmaxb], writes=[kmaxb])

        self_sb = [0]
        self_tb = [0]
        gt_i = 0
        for i in range(2):
            B.op("dve", lambda e, i=i: e.memset(Vh[i][:, :, HD:HD + 1], 1.0), writes=[onesb])
        hq_i = 0
        at_i = 0
        for tt in range(8):
            t0 = tt * 512
            nk_t = (tt + 1) * 4
            if tt == 0:
                for c in range(4):
                    B.dma("sp", y[:, c, :], T["h2"][t0 + c * 128:t0 + (c + 1) * 128, :], owner=ybufs[c], writes=[ybufs[c]])
            emit_stats(B, C, [y[:, c, :] for c in range(4)], ybufs, stat, statb, junk)
            def b_front(c):
                gc = tt * 4 + c
                s2 = gc % 2
                qbk = (1, 2) if c % 2 == 0 else (5, 6)
                B.op("dve", lambda e, c=c, s2=s2: e.scalar_tensor_tensor(
                    out=hn[s2][:], in0=y[:, c, :], scalar=stat[:, 2, c:c + 1], in1=g1[:], op0=ALU.mult, op1=ALU.mult),
                    reads=[ybufs[c], statb, g1b], writes=[hnb[s2]])
                emit_transpose8(B, C, hn[s2], hnb[s2], 0, ident, hnTc[s2][:, :, :], [hnTcb[s2]], ident_buf=identb)
                for half in range(2):
                    def mm(e, half=half, s2=s2, bk=qbk[half]):
                        ins = None
                        for dc in range(8):
                            ins = e.matmul(C.banks[bk][:], hnTc[s2][:, dc, :], wq[:, dc, half * 512:(half + 1) * 512],
                                           start=(dc == 0), stop=(dc == 7))
                        return ins
                    B.op("pe", mm, reads=[hnTcb[s2], wqb], writes=[C.bankb[qbk[half]]])

            def b_back(c):
                gc = tt * 4 + c
                s2 = gc % 2
                qbk = (1, 2) if c % 2 == 0 else (5, 6)
                for half in range(2):
                    qb_ = qbk[half]
                    B.op("act", lambda e, half=half, s2=s2, qb_=qb_: e.copy(out=Qb[s2][:, half * 512:(half + 1) * 512], in_=C.banks[qb_][:]),
                         reads=[C.bankb[qb_]], writes=[Qbb[s2]])
                    B.op("act", lambda e, half=half, qb_=qb_: e.activation(out=sq[:], in_=C.banks[qb_][:], func=AF.Square),
                         reads=[C.bankb[qb_]], writes=[sqb])
                    B.op("dve", lambda e, half=half, c=c: e.tensor_reduce(out=qn[:, c, 0, half * 4:(half + 1) * 4],
                                                                          in_=sq[:].rearrange("p (h d) -> p h d", h=4), axis=AX.X, op=ALU.add),
                         reads=[sqb], writes=[qnb[c]])
                emit_rope_sb(B, Qb[s2][:].rearrange("p (h d) -> p h d", h=8), Qbb[s2],
                             cos[:, gc, :], sin[:, gc, :], rtmp[s2], rtmpb[s2], ropeb)
                B.op("dve", lambda e, c=c: e.tensor_tensor(out=qn[:, c, 1, :], in0=qn[:, c, 0, :], in1=kmax2[:], op=ALU.mult),
                     reads=[qnb[c], kmaxb], writes=[qnb[c]])
                B.op("act", lambda e, c=c: e.activation(out=qn[:, c, 1, :], in_=qn[:, c, 1, :], func=AF.Sqrt),
                     reads=[qnb[c]], writes=[qnb[c]])
                B.op("dve", lambda e, c=c: e.tensor_scalar(out=qn[:, c, 2, :], in0=qn[:, c, 1, :], scalar1=-SCALE, scalar2=None, op0=ALU.mult),
                     reads=[qnb[c]], writes=[qnb[c]])
                emit_transpose8(B, C, Qb[s2], Qbb[s2], 0, ident, QT[:, :, c * 128:(c + 1) * 128], [QTb[c]], ident_buf=identb)

            b_front(0)
            for c in range(1, 4):
                b_front(c)
                b_back(c - 1)
            b_back(3)
            def att0(h, j):
                nonlocal gt_i
                qc = tt * 4 + j
                b = qc // 2
                g3 = gt_i % 3
                gt_i += 1
                if b >= 4:
                    qsl = QT[:, h, j * 128:(j + 1) * 128]
                    negb = qn[:, j, 2, h:h + 1]
                    B.op("pe", lambda e, qsl=qsl, h=h: e.matmul(C.banks[3][:, 0:NBLK], qsl, kmT[:, h, :], start=True, stop=True),
                         reads=[QTb[j], kmTb], writes=[C.bankb[3]])
                    g = gt[g3]
                    B.op("dve", lambda e, g=g, b=b: e.tensor_tensor(out=g[:, 0, :], in0=C.banks[3][:, 0:NBLK], in1=past[:, b, :], op=ALU.add),
                         reads=[C.bankb[3], pastb], writes=[gtb[g3]])
                    B.op("dve", lambda e, g=g: e.max(out=g[:, 1, 0:8], in_=g[:, 0, :]), reads=[gtb[g3]], writes=[gtb[g3]])
                    B.op("dve", lambda e, g=g: e.tensor_scalar(out=g[:, 2, :], in0=g[:, 0, :], scalar1=g[:, 1, 2:3], scalar2=None, op0=ALU.is_lt),
                         reads=[gtb[g3]], writes=[gtb[g3]])
                    B.op("dve", lambda e, g=g, negb=negb: e.scalar_tensor_tensor(
                        out=g[:, 3, :], in0=g[:, 2, :], scalar=NEG, in1=negb.broadcast_to([128, NBLK]), op0=ALU.mult, op1=ALU.add),
                        reads=[gtb[g3], qnb[j]], writes=[gtb[g3]])
                return dict(h=h, j=j, g3=g3)

            def att1(ctx):
                nonlocal hq_i, at_i
                h, j, g3 = ctx["h"], ctx["j"], ctx["g3"]
                if j == 0:
                    ks = hq_i % 2
                    hq_i += 1
                    B.dma("sp", KTh[ks][:, 0:nk_t * 128], T["KT"][h, :, 0:nk_t * 128], owner=KThb[ks], writes=[KThb[ks]])
                    B.dma("sp", Vh[ks][:, 0:nk_t, 0:HD], T["Vp"][h, :, 0:nk_t, :], owner=Vhb[ks], writes=[Vhb[ks]])
                ks = (hq_i - 1) % 2
                qc = tt * 4 + j
                b = qc // 2
                sub = qc % 2
                nkc = 2 * b + sub + 1
                a = at_i % 2
                at_i += 1
                qsl = QT[:, h, j * 128:(j + 1) * 128]
                negb = qn[:, j, 2, h:h + 1]
                use_sel = b >= 4
                npieces = (nkc + 3) // 4
                for kg in reversed(range(npieces)):
                    k0 = kg * 4
                    k1 = min(k0 + 4, nkc)
                    N = (k1 - k0) * 128
                    sbk = (5, 6, 1, 2)[self_sb[0] % 4]
                    self_sb[0] += 1
                    B.op("pe", lambda e, qsl=qsl, ks=ks, k0=k0, N=N, sbk=sbk: e.matmul(
                        C.banks[sbk][:, 0:N], qsl, KTh[ks][:, k0 * 128:k0 * 128 + N], start=True, stop=True),
                        reads=[QTb[j], KThb[ks]], writes=[C.bankb[sbk]])
                    chunks = []
                    kc = k0
                    while kc < k1:
                        w = 2 if (kc // 2) < b else 1
                        chunks.append((kc, w))
                        kc += w
                    if not use_sel:
                        kend = min(k1, nkc - 1)
                        if k1 == nkc:
                            kd = nkc - 1
                            B.op("dve", lambda e, sbk=sbk, kd=kd, k0=k0, a=a: e.tensor_tensor(
                                out=dg[a][:], in0=C.banks[sbk][:, (kd - k0) * 128:(kd - k0 + 1) * 128], in1=tri[:], op=ALU.add),
                                reads=[C.bankb[sbk], trib], writes=[dgb[a]])
                            B.op("act", lambda e, kd=kd, a=a, negb=negb: e.activation(
                                out=P[a][:, kd * 128:(kd + 1) * 128], in_=dg[a][:], func=AF.Exp, scale=SCALE, bias=negb),
                                reads=[dgb[a], qnb[j]], writes=[Pb[a]])
                        if kend > k0:
                            B.op("act", lambda e, sbk=sbk, k0=k0, kend=kend, a=a, negb=negb: e.activation(
                                out=P[a][:, k0 * 128:kend * 128], in_=C.banks[sbk][:, 0:(kend - k0) * 128],
                                func=AF.Exp, scale=SCALE, bias=negb),
                                reads=[C.bankb[sbk], qnb[j]], writes=[Pb[a]])
                        continue
                    for (kc, w) in reversed(chunks):
                        n_blk = kc // 2
                        if n_blk < b:
                            bias = gt[g3][:, 3, n_blk:n_blk + 1] if use_sel else negb
                            rd = [C.bankb[sbk], qnb[j]] + ([gtb[g3]] if use_sel else [])
                            B.op("act", lambda e, sbk=sbk, kc=kc, k0=k0, bias=bias, a=a: e.activation(
                                out=P[a][:, kc * 128:(kc + 2) * 128], in_=C.banks[sbk][:, (kc - k0) * 128:(kc - k0 + 2) * 128],
                                func=AF.Exp, scale=SCALE, bias=bias),
                                reads=rd, writes=[Pb[a]])
                        elif kc == nkc - 1:
                            B.op("dve", lambda e, sbk=sbk, kc=kc, k0=k0, a=a: e.tensor_tensor(
                                out=dg[a][:], in0=C.banks[sbk][:, (kc - k0) * 128:(kc - k0 + 1) * 128], in1=tri[:], op=ALU.add),
                                reads=[C.bankb[sbk], trib], writes=[dgb[a]])
                            B.op("act", lambda e, kc=kc, a=a, negb=negb: e.activation(
                                out=P[a][:, kc * 128:(kc + 1) * 128], in_=dg[a][:], func=AF.Exp, scale=SCALE, bias=negb),
                                reads=[dgb[a], qnb[j]], writes=[Pb[a]])
                        else:
                            B.op("act", lambda e, sbk=sbk, kc=kc, k0=k0, a=a, negb=negb: e.activation(
                                out=P[a][:, kc * 128:(kc + 1) * 128], in_=C.banks[sbk][:, (kc - k0) * 128:(kc - k0 + 1) * 128],
                                func=AF.Exp, scale=SCALE, bias=negb),
                                reads=[C.bankb[sbk], qnb[j]], writes=[Pb[a]])
                ctx.update(a=a, ks=ks, nkc=nkc)
                return ctx

            def att2(ctx):
                h, j, a, ks, nkc = (ctx[k] for k in ("h", "j", "a", "ks", "nkc"))
                ngr = (nkc + 7) // 8
                for gr in range(ngr):
                    c0 = gr * 8
                    c1 = min(c0 + 8, nkc)
                    tb = 7 if (self_tb[0] % 2 == 0) else 0
                    self_tb[0] += 1
                    pb_ = C.banks[tb][:].bitcast(BF16)

                    def trp(e, pb_=pb_, c0=c0, c1=c1, a=a):
                        ins = None
                        for kc in range(c0, c1):
                            ins = e.transpose(pb_[:, (kc - c0) * 128:(kc - c0 + 1) * 128], P[a][:, kc * 128:(kc + 1) * 128], ident[:])
                        return ins

                    B.op("pe", trp, reads=[Pb[a], identb], writes=[C.bankb[tb]])
                    B.op("dve", lambda e, pb_=pb_, c0=c0, c1=c1, a=a: e.tensor_copy(
                        out=PT[a][:, c0:c1, :], in_=pb_[:, 0:(c1 - c0) * 128].rearrange("p (c t) -> p c t", t=128)),
                        reads=[C.bankb[tb]], writes=[PTb[a][gr]])

                ctx["ngr"] = ngr
                return ctx

            def att3(ctx):
                h, j, a, ks, nkc, ngr = (ctx[k] for k in ("h", "j", "a", "ks", "nkc", "ngr"))

                def pv(e, a=a, ks=ks, nkc=nkc):
                    ins = None
                    for kc in range(nkc):
                        ins = e.matmul(C.banks[4][:, 0:HD + 1], PT[a][:, kc, :], Vh[ks][:, kc, :], start=(kc == 0), stop=(kc == nkc - 1))
                    return ins

                B.op("pe", pv, reads=[PTb[a][gr] for gr in range(ngr)] + [Vhb[ks], onesb], writes=[C.bankb[4]])
                B.op("dve", lambda e, a=a: e.reciprocal(out=rs[a][:, 0:1], in_=C.banks[4][:, HD:HD + 1]), reads=[C.bankb[4]], writes=[rsb[a]])
                B.op("dve", lambda e, a=a, j=j, h=h: e.tensor_scalar(out=Osb[j][:, h * 128:(h + 1) * 128], in0=C.banks[4][:, 0:HD],
                                                                     scalar1=rs[a][:, 0:1], scalar2=None, op0=ALU.mult),
                     reads=[C.bankb[4], rsb[a]], writes=[Osbb[j]])

            def p3a(c):
                o2 = c % 2
                emit_transpose8(B, C, Osb[c], Osbb[c], 0, ident, OT[o2][:, :, :], [OTb[o2]], ident_buf=identb)

            def p3b(c):
                o2 = c % 2
                obk = (1, 2) if c % 2 == 0 else (5, 6)
                for half in range(2):
                    ob_ = obk[half]

                    def mmo(e, half=half, o2=o2, ob_=ob_):
                        ins = None
                        for dc in range(8):
                            ins = e.matmul(C.banks[ob_][:], OT[o2][:, dc, :], wo[:, dc, half * 512:(half + 1) * 512],
                                           start=(dc == 0), stop=(dc == 7))
                        return ins
                    B.op("pe", mmo, reads=[OTb[o2], wob], writes=[C.bankb[ob_]])
                    ysl = y[:, c, half * 512:(half + 1) * 512]
                    B.op("dve", lambda e, ysl=ysl, ob_=ob_: e.tensor_tensor(out=ysl, in0=C.banks[ob_][:], in1=ysl, op=ALU.add),
                         reads=[C.bankb[ob_], ybufs[c]], writes=[ybufs[c]])
                B.dma("sp", T["h3"][t0 + c * 128:t0 + (c + 1) * 128, :], y[:, c, :], owner=ybufs[c], reads=[ybufs[c]])
                if tt < 7:
                    t1 = t0 + 512
                    B.dma("sp", y[:, c, :], T["h2"][t1 + c * 128:t1 + (c + 1) * 128, :], owner=ybufs[c], writes=[ybufs[c]])
                if "o_dbg" in T:
                    B.dma("sp", T["o_dbg"][t0 + c * 128:t0 + (c + 1) * 128, :], Osb[c][:], owner=Osbb[c], reads=[Osbb[c]])

            items = [(h, j) for h in range(NH) for j in range(4)]
            n_it = len(items)
            c0s, c1s, c2s = {}, {}, {}
            for step in range(n_it + 3):
                if step < n_it:
                    c0s[step] = att0(*items[step])
                if 1 <= step <= n_it:
                    c1s[step - 1] = att1(c0s.pop(step - 1))
                if 2 <= step <= n_it + 1:
                    c2s[step - 2] = att2(c1s.pop(step - 2))
                if step >= 3:
                    att3(c2s.pop(step - 3))
                d = step - (n_it - 1)
                if 0 <= d <= 3:
                    p3a(d)
                    if d >= 1:
                        p3b(d - 1)
            p3b(3)
        B.end_stage()


def build(stages=("A1", "A2", "B", "C"), dbg=(), c_src="h3"):
    nc = bass.Bass("TRN2", target_bir_lowering=False)
    T = {}

    def inp(name, shape):
        T[name] = nc.dram_tensor(name, list(shape), F32, kind="ExternalInput").ap()

    def scratch(name, shape, dt, force_in=False):
        kind = "ExternalInput" if force_in else ("ExternalOutput" if name in dbg else "Internal")
        T[name] = nc.dram_tensor(name, list(shape), dt, kind=kind).ap()

    inp("attn_norm", (2, D))
    inp("ffn_norm", (2, D))
    inp("c_ident", (128, 128))
    if "A1" in stages:
        inp("x", (S, D))
        inp("pool_w", (1, 4, 256, 256))
        inp("pool_scale", (1, D))
        inp("ffn_w_gate", (1, D, F_DENSE))
        inp("ffn_w_up", (1, D, F_DENSE))
        inp("ffn_w_down", (1, F_DENSE, D))
        inp("c_band", (128, 12, 128))
    if "A2" in stages:
        inp("kv_norm", (D,))
        inp("wk", (D, D))
        inp("wv", (D, D))
    if "A2" in stages or "B" in stages:
        inp("c_cos", (128, NCH, 16))
        inp("c_sin", (128, NCH, 16))
        fin = ("A2" not in stages)
        kind = lambda n: "ExternalInput" if fin else ("ExternalOutput" if n in dbg else "Internal")
        T["KT"] = nc.dram_tensor("KT", [NH, HD, S], BF16, kind=kind("KT")).ap()
        T["Vp"] = nc.dram_tensor("Vp", [NH, 128, NCH, HD], BF16, kind=kind("Vp")).ap()
        T["kmT"] = nc.dram_tensor("kmT", [128, NH * NBLK], F32, kind=kind("kmT")).ap()
        T["kn2"] = nc.dram_tensor("kn2", [128, NH], F32, kind=kind("kn2")).ap()
    if "B" in stages:
        inp("wq", (1, D, D))
        inp("wo", (1, D, D))
        inp("c_tri", (128, 128))
        inp("c_past", (NBLK * NBLK,))
        if "o_dbg" in dbg:
            T["o_dbg"] = nc.dram_tensor("o_dbg", [S, D], BF16, kind="ExternalOutput").ap()
    if "C" in stages:
        inp("final_norm", (D,))
        inp("router_w", (1, D, NE))
        inp("moe_w_gate", (1, NE, D, F_MOE))
        inp("moe_w_up", (1, NE, D, F_MOE))
        inp("moe_w_down", (1, NE, F_MOE, D))
        T["out"] = nc.dram_tensor("out", [S, D], F32, kind="ExternalOutput").ap()
        if "gates" in dbg:
            scratch("gates", (S, NE), F32)
    scratch("h2", (S, D), F32, force_in=("A1" not in stages and ("A2" in stages or "B" in stages or c_src == "h2")))
    if "h1" in dbg:
        scratch("h1", (S, D), F32)
    if "B" in stages or "C" in stages:
        scratch("h3", (S, D), F32, force_in=("B" not in stages and "C" in stages and c_src == "h3"))
    B = Builder(nc)
    if "A1" in stages:
        stage_A1(nc, B, T)
    if "A2" in stages:
        stage_A2(nc, B, T)
    if "B" in stages:
        stage_B(nc, B, T)
    if "C" in stages:
        stage_C(nc, B, T, src=c_src)
    return nc, T


_WEIGHT_KEYS = ("attn_norm", "ffn_norm", "pool_w", "pool_scale", "wq", "wo", "kv_norm", "wk", "wv",
                "ffn_w_gate", "ffn_w_up", "ffn_w_down", "router_w", "moe_w_gate", "moe_w_up", "moe_w_down",
                "final_norm")


def kernel(**inputs):
    n_cores = 8
    nc, T = build(stages=("A1", "A2", "B", "C"))
    consts = make_consts()
    shared = {k: np.ascontiguousarray(np.asarray(inputs[k], dtype=np.float32)) for k in _WEIGHT_KEYS}
    shared.update(consts)
    x = np.asarray(inputs["x"], dtype=np.float32)
    in_maps = []
    for b in range(n_cores):
        m = dict(shared)
        m["x"] = np.ascontiguousarray(x[b])
        in_maps.append(m)
    res = run_bass_kernel_spmd(nc, in_maps, core_ids=list(range(n_cores)))
    out = np.stack([np.asarray(res.results[b]["out"], dtype=np.float32) for b in range(n_cores)], axis=0)
    return out
```
